# Optimizing a Trainium2 kernel written in Bass

```python
import jax
import jax.numpy as jnp
from jax import lax

D_MODEL = 1024
BATCH = 8
SEQ = 4096
DEPTH = 2

GRID_W = 64
CTX_LEN = 256
HEAD_DIM = 64
ATT_HEADS = D_MODEL // 256
ATT_KV_HEADS = ATT_HEADS // 2
WIN_HEADS = D_MODEL // 256
WIN_KV_HEADS = WIN_HEADS // 2
RWKV_HEADS = D_MODEL // 128
WINDOW = 128
Q_BLOCK = 128
ROPE_THETA = 10000.0
DECAY_LORA = 64
ICLR_LORA = 64
GATE_LORA = 128
N_EXPERTS = 16
N_EXPERT_GROUPS = 4
GROUP_SCORE_K = 2
TOP_K = 2
EXPERT_FF = D_MODEL
EXPERT_BLOCK = 128
N_MOD = 6
NORM_EPS = 1e-6
GN_EPS = 64e-5

ATT_Q = ATT_HEADS * HEAD_DIM
ATT_KV = ATT_KV_HEADS * HEAD_DIM
WIN_Q = WIN_HEADS * HEAD_DIM
WIN_KV = WIN_KV_HEADS * HEAD_DIM
RWKV_WIDTH = RWKV_HEADS * HEAD_DIM
MIX_WIDTH = ATT_Q + WIN_Q + RWKV_WIDTH
RWKV_COLS = 3 * RWKV_WIDTH + 2 * DECAY_LORA + 2 * ICLR_LORA + GATE_LORA
IN_COLS = ATT_Q + 2 * ATT_KV + WIN_Q + 2 * WIN_KV + RWKV_COLS

kernel_name = 'hybrid_flow_gqa_swa_rwkv7_moe'


def _split(t, sizes):
    offs, acc = [], 0
    for s in sizes[:-1]:
        acc += s
        offs.append(acc)
    return jnp.split(t, offs, axis=-1)


def _rms_norm(x, g):
    xf = x.astype(jnp.float32)
    y = xf * lax.rsqrt(jnp.mean(xf * xf, axis=-1, keepdims=True) + NORM_EPS)
    return (y * g.astype(jnp.float32)).astype(x.dtype)


def _modulate(x, shift, scale):
    return x * (1 + scale) + shift


def _rope_tables(n_tokens):
    rows = n_tokens // GRID_W
    row = jnp.repeat(jnp.arange(rows, dtype=jnp.float32), GRID_W)
    col = jnp.tile(jnp.arange(GRID_W, dtype=jnp.float32), rows)
    n_freq = HEAD_DIM // 4
    inv = ROPE_THETA ** (-jnp.arange(n_freq, dtype=jnp.float32) / n_freq)
    ang = jnp.concatenate([row[:, None] * inv, col[:, None] * inv], axis=-1)
    return jnp.cos(ang), jnp.sin(ang)


def _apply_rope(x, cos, sin):
    c = cos[None, :, None, :].astype(x.dtype)
    s = sin[None, :, None, :].astype(x.dtype)
    x1, x2 = x[..., 0::2], x[..., 1::2]
    return jnp.stack([x1 * c - x2 * s, x1 * s + x2 * c], axis=-1).reshape(x.shape)


def _heads(t, n):
    return t.reshape(t.shape[:-1] + (n, HEAD_DIM))


def _group(q, n_kv):
    return q.reshape(q.shape[:-2] + (n_kv, q.shape[-2] // n_kv, HEAD_DIM))


def _attend(q, k, v, mask=None, sink=None):
    s = jnp.einsum('bqhgd,bkhd->bhgqk', q, k).astype(jnp.float32) * (HEAD_DIM ** -0.5)
    if mask is not None:
        s = jnp.where(mask, s, -jnp.inf)
    if sink is not None:
        col = jnp.broadcast_to(sink.astype(jnp.float32)[None, :, :, None, None], s.shape[:-1] + (1,))
        s = jnp.concatenate([s, col], axis=-1)
    p = jax.nn.softmax(s, axis=-1)
    if sink is not None:
        p = p[..., :-1]
    return jnp.einsum('bhgqk,bkhd->bqhgd', p.astype(v.dtype), v)


def _global_gqa(q, k, v, qc, kc, vc, q_g, k_g, cos, sin, ctx_out):
    B, S = q.shape[:2]
    q = _group(_apply_rope(_rms_norm(_heads(q, ATT_HEADS), q_g), cos, sin), ATT_KV_HEADS)
    k = _apply_rope(_rms_norm(_heads(k, ATT_KV_HEADS), k_g), cos, sin)
    kc = _rms_norm(_heads(kc, ATT_KV_HEADS), k_g)
    vc = _heads(vc, ATT_KV_HEADS)
    keys = jnp.concatenate([kc, k], axis=1)
    vals = jnp.concatenate([vc, _heads(v, ATT_KV_HEADS)], axis=1)
    nb = S // Q_BLOCK
    qb = jnp.moveaxis(q.reshape((B, nb, Q_BLOCK) + q.shape[2:]), 1, 0)
    o = lax.map(lambda qi: _attend(qi, keys, vals), qb)
    o = jnp.moveaxis(o, 0, 1).reshape(B, S, ATT_Q)
    o_c = None
    if ctx_out:
        qc = _group(_rms_norm(_heads(qc, ATT_HEADS), q_g), ATT_KV_HEADS)
        o_c = _attend(qc, kc, vc).reshape(B, kc.shape[1], ATT_Q)
    return o, o_c


def _window_gqa(q, k, v, qc, kc, vc, sink, cos, sin, ctx_out):
    B, S = q.shape[:2]
    n_ctx = kc.shape[1]
    q = _group(_apply_rope(_heads(q, WIN_HEADS), cos, sin), WIN_KV_HEADS)
    k = _apply_rope(_heads(k, WIN_KV_HEADS), cos, sin)
    v = _heads(v, WIN_KV_HEADS)
    kc, vc = _heads(kc, WIN_KV_HEADS), _heads(vc, WIN_KV_HEADS)
    sink = sink.reshape(WIN_KV_HEADS, WIN_HEADS // WIN_KV_HEADS)
    pad = ((0, 0), (Q_BLOCK, Q_BLOCK), (0, 0), (0, 0))
    kp, vp = jnp.pad(k, pad), jnp.pad(v, pad)
    nb = S // Q_BLOCK
    band = 3 * Q_BLOCK
    qb = jnp.moveaxis(q.reshape((B, nb, Q_BLOCK) + q.shape[2:]), 1, 0)
    ctx_mask = jnp.ones((Q_BLOCK, n_ctx), dtype=bool)

    def block(args):
        qi, n = args
        kb = lax.dynamic_slice_in_dim(kp, n * Q_BLOCK, band, axis=1)
        vb = lax.dynamic_slice_in_dim(vp, n * Q_BLOCK, band, axis=1)
        qpos = n * Q_BLOCK + jnp.arange(Q_BLOCK)
        kpos = (n - 1) * Q_BLOCK + jnp.arange(band)
        near = (jnp.abs(kpos[None, :] - qpos[:, None]) <= WINDOW) & (kpos >= 0)[None, :] & (kpos < S)[None, :]
        mask = jnp.concatenate([near, ctx_mask], axis=-1)
        return _attend(qi, jnp.concatenate([kb, kc], axis=1), jnp.concatenate([vb, vc], axis=1), mask, sink)

    o = lax.map(block, (qb, jnp.arange(nb)))
    o = jnp.moveaxis(o, 0, 1).reshape(B, S, WIN_Q)
    o_c = None
    if ctx_out:
        qc = _group(_heads(qc, WIN_HEADS), WIN_KV_HEADS)
        o_c = _attend(qc, kc, vc, sink=sink).reshape(B, n_ctx, WIN_Q)
    return o, o_c


def _centred_shift(t, w):
    tp = jnp.pad(t, ((0, 0), (1, 1), (0, 0)))
    return tp[:, :-2] * w[0] + tp[:, 1:-1] * w[1] + tp[:, 2:] * w[2]


def _wkv7(s0, decay, k, v, kk, a, r):
    emit = r is not None
    xs = [decay, k, v, kk, a] + ([r] if emit else [])
    xs = [jnp.moveaxis(z, 1, 0) for z in xs]

    def step(S, inp):
        w_t, k_t, v_t, kk_t, a_t = inp[:5]
        sa = jnp.einsum('bhvk,bhk->bhv', S, kk_t)
        S = S * w_t[:, :, None, :] - sa[..., None] * (kk_t * a_t)[:, :, None, :] + v_t[..., None] * k_t[:, :, None, :]
        y = jnp.einsum('bhvk,bhk->bhv', S, inp[5]) if emit else None
        return S, y

    S, ys = lax.scan(step, s0, xs)
    return S, (jnp.moveaxis(ys, 0, 1) if emit else None)


def _bidir_wkv7(s0_f, s0_b, decay, kdir, v, kk, a, r):
    flip = lambda z: z[:, ::-1]
    s_f, o_f = _wkv7(s0_f, decay[:, :, 0], kdir[:, :, 0], v, kk, a[:, :, 0], r)
    s_b, o_b = _wkv7(s0_b, flip(decay[:, :, 1]), flip(kdir[:, :, 1]), flip(v), flip(kk), flip(a[:, :, 1]),
                     None if r is None else flip(r))
    out = None if r is None else o_f + flip(o_b)
    return s_f, s_b, out


def _rwkv7(u, uc, conv_w, w0, w2, a0, a2, g2, k_k, k_a, r_k, ln_w, ln_b, ctx_out):
    out_dtype = u.dtype
    f32 = jnp.float32
    conv_w, w0, w2, a0, a2, g2 = (p.astype(f32) for p in (conv_w, w0, w2, a0, a2, g2))
    k_k, k_a, r_k, ln_w, ln_b = (p.astype(f32) for p in (k_k, k_a, r_k, ln_w, ln_b))
    sizes = (RWKV_WIDTH, RWKV_WIDTH, RWKV_WIDTH, 2 * DECAY_LORA, 2 * ICLR_LORA, GATE_LORA)
    hd = lambda z: _heads(z, RWKV_HEADS)

    def prep(t):
        t = _centred_shift(t.astype(f32), conv_w)
        r, k, v, wd, ad, gd = _split(t, sizes)
        lead = t.shape[:2]
        wd = wd.reshape(lead + (2, DECAY_LORA))
        ad = ad.reshape(lead + (2, ICLR_LORA))
        logw = -jax.nn.softplus(-(w0 + jnp.einsum('btjr,jrc->btjc', jnp.tanh(wd), w2))) - 0.5
        decay = jnp.exp(-jnp.exp(logw))
        a = jax.nn.sigmoid(a0 + jnp.einsum('btjr,jrc->btjc', ad, a2))
        kdir = k[:, :, None, :] * (1.0 + (a - 1.0) * k_a)
        kk = hd(k * k_k)
        kk = kk / jnp.maximum(jnp.linalg.norm(kk, axis=-1, keepdims=True), 1e-12)
        return hd(r), hd(v), kk, hd(decay), hd(a), hd(kdir), gd

    def output(o, r, v, kdir, gd):
        mu = jnp.mean(o, axis=-1, keepdims=True)
        var = jnp.mean(jnp.square(o - mu), axis=-1, keepdims=True)
        lead = o.shape[:2]
        on = ((o - mu) * lax.rsqrt(var + GN_EPS)).reshape(lead + (RWKV_WIDTH,)) * ln_w + ln_b
        bonus = jnp.sum(r[:, :, None] * kdir * r_k, axis=-1, keepdims=True).sum(axis=2) * v
        gate = jax.nn.sigmoid(gd) @ g2
        return ((on + bonus.reshape(lead + (RWKV_WIDTH,))) * gate).astype(out_dtype)

    zero = jnp.zeros((u.shape[0], RWKV_HEADS, HEAD_DIM, HEAD_DIM), f32)
    rc, vc, kkc, dc, ac, kdc, gdc = prep(uc)
    s_f, s_b, oc = _bidir_wkv7(zero, zero, dc, kdc, vc, kkc, ac, rc if ctx_out else None)
    o_ctx = output(oc, rc, vc, kdc, gdc) if ctx_out else None
    r, v, kk, dec, a, kdir, gd = prep(u)
    _, _, ol = _bidir_wkv7(s_f, s_b, dec, kdir, v, kk, a, r)
    return output(ol, r, v, kdir, gd), o_ctx


def _moe(xf, w_router, router_bias, wg, wu, wd):
    n_tok, d = xf.shape
    per_group = N_EXPERTS // N_EXPERT_GROUPS
    scores = jax.nn.sigmoid(jnp.dot(xf.astype(jnp.float32), w_router.astype(jnp.float32)))
    biased = (scores + router_bias.astype(jnp.float32)).reshape(n_tok, N_EXPERT_GROUPS, per_group)
    group_score = lax.top_k(biased, GROUP_SCORE_K)[0].sum(axis=-1)
    g_sel = jnp.argmax(group_score, axis=-1).astype(jnp.int32)
    in_group = jnp.take_along_axis(biased, g_sel[:, None, None], axis=1)[:, 0]
    local = lax.top_k(in_group, TOP_K)[1].astype(jnp.int32)
    expert = g_sel[:, None] * per_group + local
    gate = jnp.take_along_axis(scores, expert, axis=1)
    gate = gate / jnp.sum(gate, axis=-1, keepdims=True)
    n_asg = n_tok * TOP_K
    e_flat = expert.reshape(-1)
    tok_flat = jnp.repeat(jnp.arange(n_tok, dtype=jnp.int32), TOP_K)
    order = jnp.argsort(e_flat)
    e_sorted = e_flat[order]
    counts = jnp.zeros((N_EXPERTS,), jnp.int32).at[e_flat].add(1)
    padded = (counts + EXPERT_BLOCK - 1) // EXPERT_BLOCK * EXPERT_BLOCK
    start = jnp.cumsum(counts) - counts
    pend = jnp.cumsum(padded)
    pstart = pend - padded
    slot_sorted = pstart[e_sorted] + jnp.arange(n_asg, dtype=jnp.int32) - start[e_sorted]
    n_slots = -(-n_asg // EXPERT_BLOCK) * EXPERT_BLOCK + N_EXPERTS * EXPERT_BLOCK
    n_blocks = n_slots // EXPERT_BLOCK
    slot_tok = jnp.full((n_slots,), n_tok, jnp.int32).at[slot_sorted].set(tok_flat[order])
    blk_expert = jnp.minimum(
        jnp.searchsorted(pend, jnp.arange(n_blocks, dtype=jnp.int32) * EXPERT_BLOCK, side='right'),
        N_EXPERTS - 1).astype(jnp.int32)
    x_pad = jnp.concatenate([xf, jnp.zeros((1, d), xf.dtype)], axis=0)
    xb = x_pad[slot_tok].reshape(n_blocks, EXPERT_BLOCK, d)

    def expert_ffn(args):
        xi, e = args
        return (jax.nn.silu(xi @ wg[e]) * (xi @ wu[e])) @ wd[e]

    yb = lax.map(expert_ffn, (xb, blk_expert)).reshape(n_slots, d)
    slot = jnp.zeros((n_asg,), jnp.int32).at[order].set(slot_sorted).reshape(n_tok, TOP_K)
    return jnp.einsum('tkd,tk->td', yb[slot], gate.astype(xf.dtype))


def setup_inputs(seed: int = 0) -> dict:
    key = jax.random.key(seed)
    keys = iter(jax.random.split(key, 32))

    def normal(shape, scale):
        return jax.random.normal(next(keys), shape, jnp.float32) * scale

    L, D, C, E, F = DEPTH, D_MODEL, RWKV_WIDTH, N_EXPERTS, EXPERT_FF
    shift_taps = jnp.array([0.2, 0.6, 0.2], jnp.float32)[None, :, None]
    return {
        'x': normal((BATCH, SEQ, D), 1.0),
        'c': normal((BATCH, D), 1.0),
        'ctx': normal((BATCH, CTX_LEN, D), 1.0),
        'c_ctx': normal((D,), 1.0),
        'w_mod': normal((L, D, N_MOD * D), 0.5 * D ** -0.5),
        'b_mod': normal((L, N_MOD * D), 0.02),
        'norm_mix_g': 1.0 + normal((L, D), 0.02),
        'norm_ffn_g': 1.0 + normal((L, D), 0.02),
        'w_in': normal((L, D, IN_COLS), D ** -0.5),
        'q_norm_g': 1.0 + normal((L, HEAD_DIM), 0.02),
        'k_norm_g': 1.0 + normal((L, HEAD_DIM), 0.02),
        'sink_logit': normal((L, WIN_HEADS), 0.5),
        'rwkv_conv': shift_taps + normal((L, 3, RWKV_COLS), 0.05),
        'rwkv_w0': jax.random.uniform(next(keys), (L, 2, C), jnp.float32, -4.0, 1.0),
        'rwkv_w2': normal((L, 2, DECAY_LORA, C), 0.1),
        'rwkv_a0': normal((L, 2, C), 0.5),
        'rwkv_a2': normal((L, 2, ICLR_LORA, C), 0.1),
        'rwkv_g2': normal((L, GATE_LORA, C), GATE_LORA ** -0.5),
        'rwkv_k_k': 0.85 + normal((L, C), 0.05),
        'rwkv_k_a': 1.0 + normal((L, C), 0.05),
        'rwkv_r_k': normal((L, 2, RWKV_HEADS, HEAD_DIM), 0.1),
        'rwkv_ln_w': 1.0 + normal((L, C), 0.02),
        'rwkv_ln_b': normal((L, C), 0.02),
        'w_out': normal((L, MIX_WIDTH, D), MIX_WIDTH ** -0.5),
        'w_router': normal((D, E), D ** -0.5),
        'router_bias': normal((E,), 0.01),
        'e_gate': normal((L, E, D, F), D ** -0.5),
        'e_up': normal((L, E, D, F), D ** -0.5),
        'e_down': normal((L, E, F, D), F ** -0.5),
        'final_norm_g': 1.0 + normal((D,), 0.02),
    }


def reference(x, c, ctx, c_ctx, w_mod, b_mod, norm_mix_g, norm_ffn_g, w_in, q_norm_g, k_norm_g,
              sink_logit, rwkv_conv, rwkv_w0, rwkv_w2, rwkv_a0, rwkv_a2, rwkv_g2, rwkv_k_k, rwkv_k_a,
              rwkv_r_k, rwkv_ln_w, rwkv_ln_b, w_out, w_router, router_bias, e_gate, e_up, e_down,
              final_norm_g):
    B, S, D = x.shape
    n_ctx = ctx.shape[1]
    cos, sin = _rope_tables(S)
    in_sizes = (ATT_Q, ATT_KV, ATT_KV, WIN_Q, WIN_KV, WIN_KV, RWKV_COLS)
    silu_c = jax.nn.silu(c)
    silu_cc = jax.nn.silu(c_ctx)
    h, hc = x, ctx
    for l in range(DEPTH):
        ctx_out = l < DEPTH - 1
        m = jnp.split((silu_c @ w_mod[l] + b_mod[l])[:, None, :], N_MOD, axis=-1)
        mc = jnp.split(silu_cc @ w_mod[l] + b_mod[l], N_MOD, axis=-1)
        u = _split(_modulate(_rms_norm(h, norm_mix_g[l]), m[0], m[1]) @ w_in[l], in_sizes)
        uc = _split(_modulate(_rms_norm(hc, norm_mix_g[l]), mc[0], mc[1]) @ w_in[l], in_sizes)
        oa, oa_c = _global_gqa(u[0], u[1], u[2], uc[0], uc[1], uc[2], q_norm_g[l], k_norm_g[l],
                               cos, sin, ctx_out)
        ob, ob_c = _window_gqa(u[3], u[4], u[5], uc[3], uc[4], uc[5], sink_logit[l], cos, sin, ctx_out)
        oc, oc_c = _rwkv7(u[6], uc[6], rwkv_conv[l], rwkv_w0[l], rwkv_w2[l], rwkv_a0[l], rwkv_a2[l],
                          rwkv_g2[l], rwkv_k_k[l], rwkv_k_a[l], rwkv_r_k[l], rwkv_ln_w[l], rwkv_ln_b[l],
                          ctx_out)
        h = h + m[2] * (jnp.concatenate([oa, ob, oc], axis=-1) @ w_out[l])
        f = _modulate(_rms_norm(h, norm_ffn_g[l]), m[3], m[4]).reshape(B * S, D)
        if ctx_out:
            hc = hc + mc[2] * (jnp.concatenate([oa_c, ob_c, oc_c], axis=-1) @ w_out[l])
            fc = _modulate(_rms_norm(hc, norm_ffn_g[l]), mc[3], mc[4]).reshape(B * n_ctx, D)
            y = _moe(jnp.concatenate([f, fc], axis=0), w_router, router_bias, e_gate[l], e_up[l], e_down[l])
            h = h + m[5] * y[:B * S].reshape(B, S, D)
            hc = hc + mc[5] * y[B * S:].reshape(B, n_ctx, D)
        else:
            y = _moe(f, w_router, router_bias, e_gate[l], e_up[l], e_down[l])
            h = h + m[5] * y.reshape(B, S, D)
    return _rms_norm(h, final_norm_g)
```

```python
import numpy as np
import concourse.bass as bass
import concourse.mybir as mybir
from concourse.bass_utils import run_bass_kernel_spmd
from contextlib import ExitStack

F32 = mybir.dt.float32
BF16 = mybir.dt.bfloat16
AF = mybir.ActivationFunctionType
ALU = mybir.AluOpType
AX = mybir.AxisListType

SAME_ENGINE_SYNC = True
N_DMA_SEMS = 8
NT = 34
T = 4352
DEPTH = 2
EXPD = 0.6065306597126334
KT_LIMIT = None
DBG_CUT = None
SKIP_D = False
E_CUT = None
GTL_OVERRIDE = None
E_VAR = 0


class Res:
    __slots__ = ("name", "lw", "rd")

    def __init__(self, name):
        self.name = name
        self.lw = None
        self.rd = []


class Ins:
    __slots__ = ("eng", "idx", "fn", "deps", "clk", "need_inc", "semkey", "val", "is_dma")


class Sched:
    def __init__(self, nc):
        self.nc = nc
        self.q = {e: [] for e in ("pe", "act", "dve", "pool", "sp")}
        self.known = {e: {} for e in self.q}
        self.cnt = {}
        self.last_on_sem = {}
        self.dma_rr = {"sp": 0, "pool": 0, "act": 0}
        self.final = []

    def _issue(self, eng, fn, reads, writes, semkey, is_dma):
        ins = Ins()
        ins.eng = eng
        ins.fn = fn
        ins.is_dma = is_dma
        ins.need_inc = is_dma
        ins.semkey = semkey
        self.cnt[semkey] = self.cnt.get(semkey, 0) + 1
        ins.idx = self.cnt[semkey]
        deps = []
        for r in reads:
            if r.lw is not None:
                deps.append(r.lw)
        for w in writes:
            if w.lw is not None:
                deps.append(w.lw)
            deps.extend(w.rd)
        if is_dma:
            prev = self.last_on_sem.get(semkey)
            if prev is not None:
                deps.append(prev)
            self.last_on_sem[semkey] = ins
        known = self.known[eng]
        need = {}
        for d in deps:
            if d is ins:
                continue
            if (not d.is_dma) and d.eng == eng:
                if eng == "pe" or not SAME_ENGINE_SYNC:
                    continue
            if known.get(d.semkey, 0) >= d.idx:
                continue
            if need.get(d.semkey) is None or need[d.semkey].idx < d.idx:
                need[d.semkey] = d
        ins.deps = list(need.values())
        for d in ins.deps:
            d.need_inc = True
            for k, v in d.clk.items():
                if known.get(k, 0) < v:
                    known[k] = v
            if known.get(d.semkey, 0) < d.idx:
                known[d.semkey] = d.idx
        ins.clk = dict(known)
        for r in reads:
            r.rd.append(ins)
        for w in writes:
            w.lw = ins
            w.rd = []
        self.q[eng].append(ins)
        return ins

    def op(self, eng, fn, reads=(), writes=()):
        return self._issue(eng, fn, reads, writes, eng, False)

    def dma(self, fn, reads=(), writes=(), queue="sp"):
        k = self.dma_rr[queue]
        self.dma_rr[queue] = (k + 1) % N_DMA_SEMS
        return self._issue(queue, fn, reads, writes, "dma_%s_%d" % (queue, k), True)

    def I(self, eng, meth, reads=(), writes=(), **kw):
        return self.op(eng, lambda e: getattr(e, meth)(**kw), reads, writes)

    def D(self, out, in_, reads=(), writes=(), queue="sp"):
        return self.dma(lambda e: e.dma_start(out=out, in_=in_), reads, writes, queue)

    def barrier(self):
        lasts = []
        for eng, lst in self.q.items():
            for ins in reversed(lst):
                if ins.fn is not None and not ins.is_dma:
                    lasts.append(ins)
                    break
        lasts.extend(self.last_on_sem.values())
        for eng in self.q:
            ins = Ins()
            ins.eng = eng
            ins.fn = None
            ins.is_dma = False
            ins.need_inc = False
            ins.semkey = eng
            ins.idx = self.cnt.get(eng, 0)
            known = self.known[eng]
            ins.deps = []
            for d in lasts:
                if (not d.is_dma) and d.eng == eng:
                    continue
                if known.get(d.semkey, 0) >= d.idx:
                    continue
                ins.deps.append(d)
                d.need_inc = True
            for d in ins.deps:
                for k, v in d.clk.items():
                    if known.get(k, 0) < v:
                        known[k] = v
                if known.get(d.semkey, 0) < d.idx:
                    known[d.semkey] = d.idx
            ins.clk = dict(known)
            self.q[eng].append(ins)

    def finish(self, out_res):
        self.final = [r.lw for r in out_res if r.lw is not None]

    def replay(self):
        nc = self.nc
        semkeys = list(self.cnt.keys())
        vals = {k: 0 for k in semkeys}
        for f in self.final:
            f.need_inc = True
        for eng, lst in self.q.items():
            for ins in lst:
                if ins.need_inc:
                    vals[ins.semkey] += 16 if ins.is_dma else 1
                    ins.val = vals[ins.semkey]
        with ExitStack() as es:
            es.enter_context(nc.allow_non_contiguous_dma(reason="small strided parameter loads"))
            sems = {k: es.enter_context(nc.semaphore("s_" + k)) for k in semkeys}
            block = es.enter_context(nc.Block())
            engmap = {"pe": block.tensor, "act": block.scalar, "dve": block.vector,
                      "pool": block.gpsimd, "sp": block.sync}
            for eng, lst in self.q.items():
                final = self.final if eng == "sp" else []

                def body(e, lst=lst, final=final):
                    for ins in lst:
                        for d in ins.deps:
                            e.wait_ge(sems[d.semkey], d.val)
                        if ins.fn is None:
                            continue
                        r = ins.fn(e)
                        if ins.need_inc:
                            r.then_inc(sems[ins.semkey], 16 if ins.is_dma else 1)
                    for f in final:
                        e.wait_ge(sems[f.semkey], f.val)
                engmap[eng](body)


def host_consts():
    c = {}
    c["ident"] = np.eye(128, dtype=np.float32)
    n = 4096
    row = np.repeat(np.arange(n // 64, dtype=np.float32), 64)
    col = np.tile(np.arange(64, dtype=np.float32), n // 64)
    inv = (np.float32(10000.0) ** (-np.arange(16, dtype=np.float32) / np.float32(16))).astype(np.float32)
    ang = np.concatenate([row[:, None] * inv, col[:, None] * inv], axis=-1).astype(np.float32)
    c["rope"] = np.concatenate([np.cos(ang), np.sin(ang)], axis=-1).astype(np.float32)
    i = np.arange(128)[:, None]
    j = np.arange(128)[None, :]
    wm = np.zeros((128, 384), np.float32)
    wm[:, 0:128] = np.where(j < i, -30000.0, 0.0)
    wm[:, 256:384] = np.where(j > i, -30000.0, 0.0)
    c["winmask"] = wm
    same = (i // 64) == (j // 64)
    tri = np.zeros((4, 128, 128), np.float32)
    tri[0] = np.where(same & (i <= j), -EXPD, 0.0)
    tri[1] = np.where(same & (i >= j), -EXPD, 0.0)
    tri[2] = np.where(same & (i < j), -EXPD, 0.0)
    tri[3] = np.where(same & (i > j), -EXPD, 0.0)
    c["tri"] = np.ascontiguousarray(tri.transpose(1, 0, 2))
    ind = np.zeros((128, 2), np.float32)
    ind[0:64, 0] = -EXPD
    ind[64:128, 1] = -EXPD
    c["chunkind"] = ind
    mk = np.zeros((128, 4, 128), np.float32)
    mk[:, 0] = same & (i < j)
    mk[:, 1] = same & (i <= j)
    mk[:, 2] = same & (i > j)
    mk[:, 3] = same & (i >= j)
    c["mask4"] = mk
    sel = np.zeros((2, 2, 128), np.float32)
    sel[0, 0] = 1.0
    sel[1, 1] = 1.0
    c["sel"] = sel
    return c


CONST_SHAPES = {"ident": [128, 128], "rope": [4096, 64], "winmask": [128, 384], "tri": [128, 4, 128],
                "chunkind": [128, 2], "mask4": [128, 4, 128], "sel": [2, 2, 128]}

IN_SHAPES = {
    "x": [4096, 1024], "c": [1, 1024], "ctx": [256, 1024], "c_ctx": [1, 1024],
    "w_mod": [2, 1024, 6144], "b_mod": [2, 6144], "norm_mix_g": [2, 1024], "norm_ffn_g": [2, 1024],
    "w_in": [2, 1024, 2944], "q_norm_g": [2, 64], "k_norm_g": [2, 64], "sink_logit": [2, 4],
    "rwkv_conv": [2, 3, 1920], "rwkv_w0": [2, 2, 512], "rwkv_w2": [2, 128, 512], "rwkv_a0": [2, 2, 512],
    "rwkv_a2": [2, 128, 512], "rwkv_g2": [2, 128, 512], "rwkv_k_k": [2, 512], "rwkv_k_a": [2, 512],
    "rwkv_r_k": [2, 2, 512], "rwkv_ln_w": [2, 512], "rwkv_ln_b": [2, 512], "w_out": [2, 1024, 1024],
    "w_router": [1024, 16], "router_bias": [1, 16], "e_gate": [2, 16, 1024, 1024],
    "e_up": [2, 16, 1024, 1024], "e_down": [2, 16, 1024, 1024], "final_norm_g": [1, 1024],
}


def build(stop_after=None, dbg=False):
    nc = bass.Bass("TRN2", target_bir_lowering=False)
    I = {k: nc.dram_tensor(k, s, F32, kind="ExternalInput").ap() for k, s in IN_SHAPES.items()}
    C = {k: nc.dram_tensor("k_" + k, s, F32, kind="ExternalInput").ap() for k, s in CONST_SHAPES.items()}
    OUT = nc.dram_tensor("out", [4096, 1024], F32, kind="ExternalOutput").ap()
    skind = "ExternalOutput" if dbg else "Internal"
    H = nc.dram_tensor("H", [T, 1024], F32, kind=skind).ap()
    H1 = nc.dram_tensor("H1", [T, 1024], F32, kind=skind).ap()
    U = nc.dram_tensor("U", [T, 1920], F32, kind=skind).ap()
    UC = nc.dram_tensor("UC", [T, 1920], F32, kind=skind).ap()
    YD = [nc.dram_tensor("YF", [T, 512], F32, kind=skind).ap(), nc.dram_tensor("YB", [T, 512], F32, kind=skind).ap()]
    G1D = nc.dram_tensor("G1", [T, 512], F32, kind=skind).ap()
    G2D = nc.dram_tensor("G2", [T, 512], F32, kind=skind).ap()
    OT = nc.dram_tensor("OT", [1024, T], BF16, kind=skind).ap()
    S = Sched(nc)
    rH = [Res("H%d" % i) for i in range(NT)]
    rH1 = [Res("H1%d" % i) for i in range(NT)]
    rU = Res("U")
    rUC = [Res("UC%d" % i) for i in range(NT)]
    rY = [[Res("Y%d_%d" % (d, i)) for i in range(NT)] for d in range(2)]
    rG = [Res("G%d" % i) for i in range(NT)]
    rOT = Res("OT")
    rOUT = Res("OUT")

    def mm(out, lhsT, rhs, start, stop, reads, writes):
        S.I("pe", "matmul", reads, writes, out=out, lhsT=lhsT, rhs=rhs, start=start, stop=stop)

    def tp(out, in_, idn, reads, writes):
        S.I("pe", "transpose", reads, writes, out=out, in_=in_, identity=idn)

    def tt(eng, out, in0, in1, op, reads, writes):
        S.I(eng, "tensor_tensor", reads, writes, out=out, in0=in0, in1=in1, op=op)

    def ts(eng, out, in0, s1, s2, op0, op1, reads, writes):
        if op1 is None:
            S.I(eng, "tensor_scalar", reads, writes, out=out, in0=in0, scalar1=s1, scalar2=None, op0=op0)
        else:
            S.I(eng, "tensor_scalar", reads, writes, out=out, in0=in0, scalar1=s1, scalar2=s2, op0=op0, op1=op1)

    def stt(out, in0, scalar, in1, op0, op1, reads, writes):
        S.I("dve", "scalar_tensor_tensor", reads, writes, out=out, in0=in0, scalar=scalar, in1=in1, op0=op0, op1=op1)

    def act(out, in_, func, reads, writes, **kw):
        S.I("act", "activation", reads, writes, out=out, in_=in_, func=func, **kw)

    def cp(eng, out, in_, reads, writes):
        if eng == "act":
            S.I("act", "copy", reads, writes, out=out, in_=in_)
        else:
            S.I(eng, "tensor_copy", reads, writes, out=out, in_=in_)

    def red(out, in_, op, reads, writes, axis=AX.X, **kw):
        S.I("dve", "tensor_reduce", reads, writes, out=out, in_=in_, axis=axis, op=op, **kw)

    def hsrc(l, ti):
        if l == 0:
            return I["ctx"][ti * 128:(ti + 1) * 128, :] if ti < 2 else I["x"][(ti - 2) * 128:(ti - 1) * 128, :]
        return H[ti * 128:(ti + 1) * 128, :]

    with ExitStack() as top:
        uid = [0]

        def SB(es, name, shape, dt=F32):
            uid[0] += 1
            name = "%s_%d" % (name, uid[0])
            return es.enter_context(nc.sbuf_tensor(name, shape, dt)), Res(name)

        class Ring:
            def __init__(self, es, name, shape, dt, n):
                self.items = [SB(es, "%s%d" % (name, i), shape, dt) for i in range(n)]
                self.i = 0

            def next(self):
                it = self.items[self.i]
                self.i = (self.i + 1) % len(self.items)
                return it

        PS = [(top.enter_context(nc.psum_tensor("ps%d" % i, [128, 512], F32)), Res("ps%d" % i)) for i in range(8)]

        class Rot:
            def __init__(self, ids):
                self.ids = list(ids)
                self.i = 0

            def next(self):
                p = PS[self.ids[self.i]]
                self.i = (self.i + 1) % len(self.ids)
                return p

        def bfv(pt):
            return pt[:].bitcast(BF16)

        ident, rident = SB(top, "ident", [128, 128])
        identb, ridentb = SB(top, "identb", [128, 128], BF16)
        S.D(ident[:], C["ident"], writes=[rident])
        S.D(identb[:], C["ident"], writes=[ridentb], queue="pool")
        sel, rsel = SB(top, "sel", [2, 2, 128])
        S.D(sel[:], C["sel"], writes=[rsel])
        ones, rones = SB(top, "ones", [128, 128])
        S.I("pool", "memset", [], [rones], ap=ones[:], constant=1.0)
        cc, rcc = SB(top, "cc", [128, 8, 2])
        ccr, rccr = SB(top, "ccr", [128, 8, 2])
        S.D(ccr[:, :, 0], I["c"].rearrange("o (k p) -> p (o k)", p=128), writes=[rccr])
        S.D(ccr[:, :, 1], I["c_ctx"].rearrange("o (k p) -> p (o k)", p=128), writes=[rccr])
        act(cc[:], ccr[:], AF.Silu, [rccr], [rcc])

        for l in range(DEPTH):
            last = (l == DEPTH - 1)
            with ExitStack() as LS:
                FM, rFM = SB(LS, "FM", [128, 4, 8, 2])
                GM, rGM = SB(LS, "GM", [128, 2, 2, 1024])
                with ExitStack() as es:
                    rot = Rot(range(8))
                    R, rR = SB(es, "Rmod", [2, 6144])
                    bm, rbm = SB(es, "bm", [2, 6144])
                    g2, rg2 = SB(es, "g2p", [2, 2, 1024])
                    AB, rAB = SB(es, "AB", [2, 4, 1024])
                    S.D(bm[:], I["b_mod"][l:l + 1, :].partition_broadcast(2), writes=[rbm])
                    S.D(g2[:, 0, :], I["norm_mix_g"][l:l + 1, :].partition_broadcast(2), writes=[rg2])
                    S.D(g2[:, 1, :], I["norm_ffn_g"][l:l + 1, :].partition_broadcast(2), writes=[rg2])
                    wring = Ring(es, "wm", [128, 8, 512], F32, 2)
                    for j in range(12):
                        wm, rwm = wring.next()
                        S.D(wm[:], I["w_mod"][l, :, j * 512:(j + 1) * 512].rearrange("(k p) n -> p k n", p=128), writes=[rwm])
                        pt, rpt = rot.next()
                        for k in range(8):
                            mm(pt[0:2, :], cc[:, k, :], wm[:, k, :], k == 0, k == 7, [rcc, rwm], [rpt])
                        tt("dve", R[:, j * 512:(j + 1) * 512], pt[0:2, :], bm[:, j * 512:(j + 1) * 512], ALU.add, [rpt, rbm], [rR])
                    stt(AB[:, 0, :], R[:, 1024:2048], 1.0, g2[:, 0, :], ALU.add, ALU.mult, [rR, rg2], [rAB])
                    cp("dve", AB[:, 1, :], R[:, 0:1024], [rR], [rAB])
                    stt(AB[:, 2, :], R[:, 4096:5120], 1.0, g2[:, 1, :], ALU.add, ALU.mult, [rR, rg2], [rAB])
                    cp("dve", AB[:, 3, :], R[:, 3072:4096], [rR], [rAB])
                    pt, rpt = rot.next()
                    for v in range(4):
                        for k in range(8):
                            o = (v * 8 + k) * 2
                            tp(pt[:, o:o + 2], AB[:, v, k * 128:(k + 1) * 128], ident[0:2, 0:2], [rAB, rident], [rpt])
                    cp("dve", FM[:].rearrange("p a b c -> p (a b c)"), pt[:, 0:64], [rpt], [rFM])
                    for gi, off in ((0, 2048), (1, 5120)):
                        for w in range(2):
                            for hh in range(2):
                                pt, rpt = rot.next()
                                mm(pt[:], sel[:, w, :], R[:, off + hh * 512:off + (hh + 1) * 512], True, True, [rsel, rR], [rpt])
                                cp("act", GM[:, gi, w, hh * 512:(hh + 1) * 512], pt[:], [rpt], [rGM])
                S.barrier()
                if dbg:
                    DFM = nc.dram_tensor("DFM%d" % l, [128, 64], F32, kind="ExternalOutput").ap()
                    DGM = nc.dram_tensor("DGM%d" % l, [128, 4096], F32, kind="ExternalOutput").ap()
                    rdd = Res("dd")
                    S.D(DFM, FM[:].rearrange("p a b c -> p (a b c)"), reads=[rFM], writes=[rdd])
                    S.D(DGM, GM[:].rearrange("p a b c -> p (a b c)"), reads=[rGM], writes=[rdd])
                if stop_after == "A":
                    break

                def norm_mod_T(rg, ht, rht, which, vsel, xmT, rxmT, pbanks, fp32=False, xf=None, rxf=None):
                    junk, rjunk = rg["junk"].next()
                    st, rst = rg["st"].next()
                    act(junk[:], ht[:], AF.Square, [rht], [rjunk, rst], scale=1.0 / 32.0, accum_out=st[:, 0:1])
                    act(st[:, 1:2], st[:, 0:1], AF.Sqrt, [rst], [rst], bias=1e-6, scale=1.0)
                    S.I("dve", "reciprocal", [rst], [rst], out=st[:, 2:3], in_=st[:, 1:2])
                    if not fp32:
                        xn, rxn = rg["xn"].next()
                        ts("dve", xn[:], ht[:], st[:, 2:3], None, ALU.mult, None, [rht, rst], [rxn])
                        pt, rpt = pbanks.next()
                        pv = bfv(pt)
                        for k in range(8):
                            tp(pv[:, k * 128:(k + 1) * 128], xn[:, k * 128:(k + 1) * 128], identb[:], [rxn, ridentb], [rpt])
                        for k in range(8):
                            act(xmT[:, k, :], pv[:, k * 128:(k + 1) * 128], AF.Identity, [rpt, rFM], [rxmT],
                                scale=FM[:, vsel, k, which:which + 1], bias=FM[:, vsel + 1, k, which:which + 1])
                    else:
                        xn, rxn = rg["xnf"].next()
                        ts("dve", xn[:], ht[:], st[:, 2:3], None, ALU.mult, None, [rht, rst], [rxn])
                        for hh in range(2):
                            pt, rpt = pbanks.next()
                            for k in range(4):
                                kk = hh * 4 + k
                                tp(pt[:, k * 128:(k + 1) * 128], xn[:, kk * 128:(kk + 1) * 128], ident[:], [rxn, rident], [rpt])
                            for k in range(4):
                                kk = hh * 4 + k
                                act(xf[:, kk, :], pt[:, k * 128:(k + 1) * 128], AF.Identity, [rpt, rFM], [rxf],
                                    scale=FM[:, vsel, kk, which:which + 1], bias=FM[:, vsel + 1, kk, which:which + 1])
                        cp("dve", xmT, xf[:], [rxf], [rxmT])

                with ExitStack() as AS:
                    QT, rQT = SB(AS, "QT", [128, 2, T], BF16)
                    KT, rKT = SB(AS, "KT", [128, T], BF16)
                    VG, rVG = SB(AS, "VG", [128, NT, 2, 65], BF16)
                    QWT, rQWT = SB(AS, "QWT", [128, 2, T], BF16)
                    KWT, rKWT = SB(AS, "KWT", [128, T], BF16)
                    VW, rVW = SB(AS, "VW", [128, NT, 2, 64], BF16)
                    S.I("pool", "memset", [], [rVG], ap=VG[:], constant=1.0)
                    with ExitStack() as es:
                        Win, rWin = SB(es, "Win", [128, 8, 2944], BF16)
                        for k in range(8):
                            wsrc = I["w_in"][l, k * 128:(k + 1) * 128, :]
                            for qo in (0, 512):
                                for j in range(2):
                                    S.D(Win[:, k, qo + j * 128:qo + (j + 1) * 128].rearrange("p (g d) -> p g d", g=2),
                                        wsrc[:, qo:qo + 256].rearrange("p (g j d) -> p g j d", g=2, j=2)[:, :, j, :], writes=[rWin], queue="pool")
                            S.D(Win[:, k, 256:512], wsrc[:, 256:512], writes=[rWin], queue="pool")
                            S.D(Win[:, k, 768:2944], wsrc[:, 768:2944], writes=[rWin], queue="pool")
                        G6, rG6 = SB(es, "G6", [128, 6, 64])
                        S.D(G6[:, 0, :], I["q_norm_g"][l:l + 1, :].partition_broadcast(128), writes=[rG6])
                        S.D(G6[:, 4, :], I["k_norm_g"][l:l + 1, :].partition_broadcast(128), writes=[rG6])
                        for hh in (1, 2, 3):
                            cp("pool", G6[:, hh, :], G6[:, 0, :], [rG6], [rG6])
                        cp("pool", G6[:, 5, :], G6[:, 4, :], [rG6], [rG6])
                        rings = {"junk": Ring(es, "junk", [128, 1024], F32, 1), "st": Ring(es, "st", [128, 4], F32, 2),
                                 "xn": Ring(es, "xn", [128, 1024], BF16, 2)}
                        hring = Ring(es, "ht", [128, 1024], F32, 2)
                        xring = Ring(es, "xmT", [128, 8, 128], BF16, 2)
                        uaring = Ring(es, "ua", [128, 1024], F32, 2)
                        urring = Ring(es, "ur", [128, 1920], F32, 2)
                        csring = Ring(es, "cs", [128, 64], F32, 2)
                        qrring = Ring(es, "qr", [128, 12, 64], BF16, 2)
                        tmpring = Ring(es, "tmpb", [128, 6, 64], F32, 4)
                        sring = Ring(es, "ssb", [128, 16], F32, 2)
                        rotT = Rot([0, 1])
                        rotU = Rot([2, 3, 4, 5])
                        rotQ = Rot([6, 7])
                        pend_qk = []
                        b_tiles = list(range(NT if KT_LIMIT is None else KT_LIMIT))
                        xm_of = {}

                        def b_norm(ti_):
                            which_ = 1 if ti_ < 2 else 0
                            ht, rht = hring.next()
                            S.D(ht[:], hsrc(l, ti_), reads=[rH[ti_]], writes=[rht])
                            xm_, rxm_ = xring.next()
                            norm_mod_T(rings, ht, rht, which_, 0, xm_, rxm_, rotT)
                            xm_of[ti_] = (xm_, rxm_)
                        b_norm(b_tiles[0])
                        for ti in b_tiles:
                            which = 1 if ti < 2 else 0
                            if ti + 1 <= b_tiles[-1]:
                                b_norm(ti + 1)
                            xmT, rxmT = xm_of.pop(ti)
                            ua, rua = uaring.next()
                            ur, rur = urring.next()
                            for cch in range(6):
                                c0 = cch * 512
                                cw = min(512, 2944 - c0)
                                pt, rpt = rotU.next()
                                for k in range(8):
                                    mm(pt[:, 0:cw], xmT[:, k, :], Win[:, k, c0:c0 + cw], k == 0, k == 7, [rxmT, rWin], [rpt])
                                if cch < 2:
                                    dst, rdst = ua[:, c0:c0 + 512], rua
                                else:
                                    dst, rdst = ur[:, c0 - 1024:c0 - 1024 + cw], rur
                                cp("act" if cch % 2 == 0 else "dve", dst, pt[:, 0:cw], [rpt], [rdst])
                            S.D(U[ti * 128:(ti + 1) * 128, :], ur[:], reads=[rur], writes=[Res("u")])
                            if DBG_CUT == 2:
                                continue
                            qr, rqr = qrring.next()
                            ss, rss = sring.next()
                            t1, rt1 = tmpring.next()
                            t2, rt2 = tmpring.next()
                            uq = ua[:, 0:384].rearrange("p (h d) -> p h d", d=64)
                            uw = ua[:, 512:896].rearrange("p (h d) -> p h d", d=64)
                            tt("pool", t1[:], uq, uq, ALU.mult, [rua], [rt1])
                            red(ss[:, 0:6], t1[:], ALU.add, [rt1], [rss])
                            act(ss[:, 6:12], ss[:, 0:6], AF.Sqrt, [rss], [rss], bias=1e-6, scale=1.0 / 64.0)
                            S.I("dve", "reciprocal", [rss], [rss], out=ss[:, 6:12], in_=ss[:, 6:12])
                            tt("dve", t1[:], uq, ss[:, 6:12].unsqueeze(2).to_broadcast([128, 6, 64]), ALU.mult, [rua, rss], [rt1])
                            if ti < 2:
                                tt("pool", qr[:, 0:6, :], t1[:], G6[:], ALU.mult, [rt1, rG6], [rqr])
                                cp("pool", qr[:, 6:12, :], uw, [rua], [rqr])
                            else:
                                cs, rcs = csring.next()
                                S.D(cs[:], C["rope"][(ti - 2) * 128:(ti - 1) * 128, :], writes=[rcs])
                                tt("pool", t2[:], t1[:], G6[:], ALU.mult, [rt1, rG6], [rt2])
                                for (src, rsrc, o6, eng2) in ((t2[:], rt2, 0, "dve"), (uw, rua, 6, "pool")):
                                    sv = src.rearrange("p h (i two) -> p h i two", two=2)
                                    x1, x2 = sv[:, :, :, 0], sv[:, :, :, 1]
                                    cosb = cs[:, 0:32].unsqueeze(1).to_broadcast([128, 6, 32])
                                    sinb = cs[:, 32:64].unsqueeze(1).to_broadcast([128, 6, 32])
                                    ta, rta = tmpring.next()
                                    tav = ta[:].rearrange("p h (k i) -> p h k i", k=2)
                                    ov = qr[:, o6:o6 + 6, :].rearrange("p h (i two) -> p h i two", two=2)
                                    tt(eng2, tav[:, :, 0, :], x1, cosb, ALU.mult, [rsrc, rcs], [rta])
                                    tt(eng2, tav[:, :, 1, :], x2, sinb, ALU.mult, [rsrc, rcs], [rta])
                                    tt(eng2, ov[:, :, :, 0], tav[:, :, 0, :], tav[:, :, 1, :], ALU.subtract, [rta], [rqr])
                                    tt(eng2, tav[:, :, 0, :], x1, sinb, ALU.mult, [rsrc, rcs, rta], [rta])
                                    tt(eng2, tav[:, :, 1, :], x2, cosb, ALU.mult, [rsrc, rcs, rta], [rta])
                                    tt(eng2, ov[:, :, :, 1], tav[:, :, 0, :], tav[:, :, 1, :], ALU.add, [rta], [rqr])
                            cp("pool", VG[:, ti, :, 0:64], ua[:, 384:512].rearrange("p (g d) -> p g d", d=64), [rua], [rVG])
                            cp("pool", VW[:, ti, :, :], ua[:, 896:1024].rearrange("p (g d) -> p g d", d=64), [rua], [rVW])
                            def qk_T(qr=qr, rqr=rqr, ti=ti):
                                pt, rpt = rotQ.next()
                                pv = bfv(pt)
                                for o6, tq in ((0, 0), (6, 3)):
                                    for j in range(2):
                                        tp(pv[:, (tq + j) * 128:(tq + j + 1) * 128], qr[:, o6 + 2 * j:o6 + 2 * j + 2, :].rearrange("p h d -> p (h d)"), identb[:], [rqr, ridentb], [rpt])
                                    tp(pv[:, (tq + 2) * 128:(tq + 3) * 128], qr[:, o6 + 4:o6 + 6, :].rearrange("p h d -> p (h d)"), identb[:], [rqr, ridentb], [rpt])
                                tcols = slice(ti * 128, (ti + 1) * 128)
                                cp("act", QT[:, :, tcols], pv[:, 0:256].rearrange("p (j t) -> p j t", j=2), [rpt], [rQT])
                                cp("act", KT[:, tcols], pv[:, 256:384], [rpt], [rKT])
                                cp("act", QWT[:, :, tcols], pv[:, 384:640].rearrange("p (j t) -> p j t", j=2), [rpt], [rQWT])
                                cp("act", KWT[:, tcols], pv[:, 640:768], [rpt], [rKWT])
                            for fn_ in pend_qk:
                                fn_()
                            pend_qk[:] = [qk_T]
                        for fn_ in pend_qk:
                            fn_()
                    S.barrier()
                    if stop_after == "B":
                        if dbg and KT_LIMIT != 0 and DBG_CUT is None:
                            ncl = (NT if KT_LIMIT is None else KT_LIMIT) * 128
                            DQT = nc.dram_tensor("DQT", [128, 2, T], BF16, kind="ExternalOutput").ap()
                            DKT = nc.dram_tensor("DKT", [128, T], BF16, kind="ExternalOutput").ap()
                            DQW = nc.dram_tensor("DQW", [128, 2, T], BF16, kind="ExternalOutput").ap()
                            DKW = nc.dram_tensor("DKW", [128, T], BF16, kind="ExternalOutput").ap()
                            rdd = Res("dd2")
                            S.D(DQT[:, :, 0:ncl], QT[:, :, 0:ncl], reads=[rQT], writes=[rdd])
                            S.D(DKT[:, 0:ncl], KT[:, 0:ncl], reads=[rKT], writes=[rdd])
                            S.D(DQW[:, :, 0:ncl], QWT[:, :, 0:ncl], reads=[rQWT], writes=[rdd])
                            S.D(DKW[:, 0:ncl], KWT[:, 0:ncl], reads=[rKWT], writes=[rdd])
                        break
                    with ExitStack() as es:
                      CW, rCW = SB(es, "CW", [128, 3, 1920])
                      S.D(CW[:].rearrange("p a b -> p (a b)"), I["rwkv_conv"][l:l + 1].rearrange("o a b -> o (a b)").partition_broadcast(128), writes=[rCW])
                      e0_ust = [SB(es, "ust%d_" % i, [128, 1920]) for i in range(3)]
                      e0_ucr = Ring(es, "ucv0_", [128, 1920], F32, 2)
                      e0_tiles = list(range(NT if KT_LIMIT is None else KT_LIMIT - 1))
                      e0_state = {}

                      def e0_stage1(ti):
                          first_of_seq = ti in (0, 2)
                          last_of_seq = ti in (1, 33)
                          uc, ruc = e0_ucr.next()
                          e0_state[ti] = (uc, ruc)
                          for jj, off in ((0, -1), (1, 0), (2, 1)):
                              u_, ru_ = e0_ust[jj]
                              if off == -1 and first_of_seq:
                                  S.I("pool", "memset", [], [ru_], ap=u_[:], constant=0.0)
                                  S.D(u_[1:128, :], U[ti * 128:ti * 128 + 127, :], reads=[], writes=[ru_])
                              elif off == 1 and last_of_seq:
                                  S.I("pool", "memset", [], [ru_], ap=u_[:], constant=0.0)
                                  S.D(u_[0:127, :], U[ti * 128 + 1:ti * 128 + 128, :], reads=[], writes=[ru_])
                              else:
                                  S.D(u_[:], U[ti * 128 + off:ti * 128 + off + 128, :], reads=[], writes=[ru_])
                          tt("pool", uc[:], e0_ust[0][0][:], CW[:, 0, :], ALU.mult, [e0_ust[0][1], rCW], [ruc])
                          tt("pool", e0_ust[2][0][:], e0_ust[2][0][:], CW[:, 2, :], ALU.mult, [e0_ust[2][1], rCW], [e0_ust[2][1]])

                      def e0_stage2(ti):
                          uc, ruc = e0_state.pop(ti)
                          tt("dve", e0_ust[1][0][:], e0_ust[1][0][:], CW[:, 1, :], ALU.mult, [e0_ust[1][1], rCW], [e0_ust[1][1]])
                          tt("dve", uc[:], uc[:], e0_ust[1][0][:], ALU.add, [ruc, e0_ust[1][1]], [ruc])
                          tt("dve", uc[:], uc[:], e0_ust[2][0][:], ALU.add, [ruc, e0_ust[2][1]], [ruc])
                          S.D(UC[ti * 128:(ti + 1) * 128, :], uc[:], reads=[ruc], writes=[rUC[ti]])
                      e0_sched = []
                      for t_ in e0_tiles:
                          e0_sched.append((e0_stage1, t_))
                          e0_sched.append((e0_stage2, t_))

                      def e0_step():
                          if e0_sched:
                              fn_, t_ = e0_sched.pop(0)
                              fn_(t_)
                      if SKIP_D:
                        while e0_sched:
                            e0_step()
                        zt, rzt = SB(es, "zt", [128, 4, 128], BF16)
                        S.I("pool", "memset", [], [rzt], ap=zt[:], constant=0.0)
                        for ti_ in range(KT_LIMIT):
                            S.D(OT[0:512, ti_ * 128:(ti_ + 1) * 128].rearrange("(c p) t -> p c t", p=128), zt[:], reads=[rzt], writes=[rOT])
                      if not SKIP_D:
                          nb, rnb = SB(es, "negb", [128, 4])
                          gk, rgk = SB(es, "gk", [1, 132])
                          SK, rSK = SB(es, "SK", [128, 4])
                          WM, rWM = SB(es, "WM", [128, 384])
                          S.D(WM[:], C["winmask"], writes=[rWM])
                          S.D(SK[:], I["sink_logit"][l:l + 1, :].partition_broadcast(128), writes=[rSK])
                          S.D(gk[:, 0:64], I["q_norm_g"][l:l + 1, :], writes=[rgk])
                          S.D(gk[:, 64:128], I["k_norm_g"][l:l + 1, :], writes=[rgk])
                          red(gk[:, 128:130], gk[:, 0:128].rearrange("p (a b) -> p a b", a=2), ALU.max, [rgk], [rgk], apply_absolute_value=True)
                          stt(gk[:, 130:131], gk[:, 128:129], -8.0, gk[:, 129:130], ALU.mult, ALU.mult, [rgk], [rgk])
                          pt, rpt = PS[7]
                          mm(pt[:, 0:1], ones[0:1, :], gk[0:1, 130:131], True, True, [rones, rgk], [rpt])
                          cp("dve", nb[:, 0:1], pt[:, 0:1], [rpt], [rnb])
                          pring = Ring(es, "pT", [128, 512], BF16, 5)
                          oring = Ring(es, "osb", [64, 512], F32, 2)
                          obring = Ring(es, "obf", [64, 512], BF16, 2)
                          rdring = Ring(es, "rden", [128, 512], F32, 2)
                          rotO = Rot([0, 1])
                          rotS = Rot([2, 3, 4, 5])
                          rotB = Rot([6, 7])
                          blocks = [(256 + qb * 256, list(range(NT))) for qb in range(16)]
                          if not last:
                              blocks = [(0, [0, 1])] + blocks
                          for g in range(2):
                              gs = slice(g * 64, (g + 1) * 64)
                              for (q0, kcs) in blocks:
                                  e0_step()
                                  po, rpo = rotO.next()
                                  pend = []
                                  for ci, kc in enumerate(kcs):
                                      ps_, rps_ = rotS.next()
                                      mm(ps_[:], KT[gs, kc * 128:(kc + 1) * 128], QT[gs, :, q0:q0 + 256], True, True, [rKT, rQT], [rps_])
                                      pT, rpT = pring.next()
                                      act(pT[:], ps_[:], AF.Exp, [rps_, rnb], [rpT], scale=0.125, bias=nb[:, 0:1])
                                      pend.append((ci, kc, pT, rpT))
                                      if len(pend) > 2:
                                          ci0, kc0, pT0, rpT0 = pend.pop(0)
                                          mm(po[0:65, :], VG[:, kc0, g, :], pT0[:], ci0 == 0, ci0 == len(kcs) - 1, [rVG, rpT0], [rpo])
                                  while pend:
                                      ci0, kc0, pT0, rpT0 = pend.pop(0)
                                      mm(po[0:65, :], VG[:, kc0, g, :], pT0[:], ci0 == 0, ci0 == len(kcs) - 1, [rVG, rpT0], [rpo])
                                  rd, rrd = rdring.next()
                                  osb, rosb = oring.next()
                                  obf, robf = obring.next()
                                  S.I("dve", "reciprocal", [rpo], [rrd], out=rd[64:65, :], in_=po[64:65, :])
                                  cp("act", osb[:], po[0:64, :], [rpo], [rosb])
                                  pb, rpb = rotB.next()
                                  mm(pb[0:64, :], ones[64:65, 0:64], rd[64:65, :], True, True, [rones, rrd], [rpb])
                                  tt("dve", obf[:], osb[:], pb[0:64, :], ALU.mult, [rosb, rpb], [robf])
                                  for j in range(2):
                                      hd = 2 * g + j
                                      S.D(OT[hd * 64:(hd + 1) * 64, q0:q0 + 256], obf[:, j * 256:(j + 1) * 256], reads=[robf], writes=[Res("ot")], queue="pool")
                          while e0_sched:
                              e0_step()
                          scring = Ring(es, "sc", [128, 640], F32, 2)
                          pfring = Ring(es, "pf", [128, 640], F32, 2)
                          pnring = Ring(es, "pn", [128, 640], BF16, 2)
                          ptring = Ring(es, "ptT", [128, 5, 128], BF16, 2)
                          mring = Ring(es, "mx", [128, 8], F32, 4)
                          owring = Ring(es, "ow", [64, 4, 128], BF16, 2)
                          rotA = Rot([0, 1])
                          rotB2 = Rot([2, 3])
                          rotP = Rot([4, 5])
                          rotW = Rot([6, 7])
                          qtiles = list(range(2, NT)) if last else list(range(NT))
                          pend_w = []
                          for ti in qtiles:
                              tcols = slice(ti * 128, (ti + 1) * 128)
                              n = ti - 2
                              band = [] if ti < 2 else [b for b in (n - 1, n, n + 1) if 0 <= b < 32]
                              bw = 128 * len(band)
                              Wd = bw + 256
                              ktiles = [b + 2 for b in band] + [0, 1]
                              pow_, rpow = rotW.next()
                              ow, row = owring.next()
                              for hd in range(4):
                                  g, j = hd // 2, hd % 2
                                  gs = slice(g * 64, (g + 1) * 64)
                                  sc, rsc = scring.next()
                                  if band:
                                      pA, rpA = rotA.next()
                                      k0 = (band[0] + 2) * 128
                                      mm(pA[:, 0:bw], QWT[gs, j, tcols], KWT[gs, k0:k0 + bw], True, True, [rQWT, rKWT], [rpA])
                                      m0 = 0 if band[0] == n - 1 else 128
                                      tt("dve", sc[:, 0:bw], pA[:, 0:bw], WM[:, m0:m0 + bw], ALU.add, [rpA, rWM], [rsc])
                                  pB, rpB = rotB2.next()
                                  mm(pB[:, 0:256], QWT[gs, j, tcols], KWT[gs, 0:256], True, True, [rQWT, rKWT], [rpB])
                                  cp("act", sc[:, bw:bw + 256], pB[:, 0:256], [rpB], [rsc])
                                  mx, rmx = mring.next()
                                  red(mx[:, 0:1], sc[:, 0:Wd], ALU.max, [rsc], [rmx])
                                  stt(mx[:, 1:2], mx[:, 0:1], 0.125, SK[:, hd:hd + 1], ALU.mult, ALU.max, [rmx, rSK], [rmx])
                                  ts("dve", mx[:, 2:3], mx[:, 1:2], -1.0, None, ALU.mult, None, [rmx], [rmx])
                                  pf, rpf = pfring.next()
                                  act(pf[:, 0:Wd], sc[:, 0:Wd], AF.Exp, [rsc, rmx], [rpf, rmx], scale=0.125, bias=mx[:, 2:3], accum_out=mx[:, 3:4])
                                  act(mx[:, 4:5], SK[:, hd:hd + 1], AF.Exp, [rSK, rmx], [rmx], scale=1.0, bias=mx[:, 2:3])
                                  tt("dve", mx[:, 5:6], mx[:, 3:4], mx[:, 4:5], ALU.add, [rmx], [rmx])
                                  S.I("dve", "reciprocal", [rmx], [rmx], out=mx[:, 6:7], in_=mx[:, 5:6])
                                  pn, rpn = pnring.next()
                                  ts("dve", pn[:, 0:Wd], pf[:, 0:Wd], mx[:, 6:7], None, ALU.mult, None, [rpf, rmx], [rpn])
                                  def stage_b(pn=pn, rpn=rpn, Wd=Wd, ktiles=ktiles, g=g, hd=hd, pow_=pow_, rpow=rpow, ow=ow, row=row, tcols=tcols):
                                      ptp, rptp = rotP.next()
                                      ptv = bfv(ptp)
                                      nch = Wd // 128
                                      for cidx in range(nch):
                                          tp(ptv[:, cidx * 128:(cidx + 1) * 128], pn[:, cidx * 128:(cidx + 1) * 128], identb[:], [rpn, ridentb], [rptp])
                                      ptT, rptT = ptring.next()
                                      cp("act", ptT[:, 0:nch, :], ptv[:, 0:nch * 128].rearrange("p (c t) -> p c t", t=128), [rptp], [rptT])
                                      for cidx in range(nch):
                                          kt = ktiles[cidx]
                                          mm(pow_[0:64, hd * 128:(hd + 1) * 128], VW[:, kt, g, :], ptT[:, cidx, :], cidx == 0, cidx == nch - 1, [rVW, rptT], [rpow])
                                      if hd == 3:
                                          cp("dve", ow[:].rearrange("p h t -> p (h t)"), pow_[0:64, :], [rpow], [row])
                                          S.D(OT[256:512, tcols].rearrange("(h d) t -> d h t", d=64), ow[:], reads=[row], writes=[Res("ot")], queue="pool")
                                  for fn_ in pend_w:
                                      fn_()
                                  pend_w[:] = [stage_b]
                          for fn_ in pend_w:
                              fn_()
                S.barrier()
                if stop_after == "D":
                    break
                with ExitStack() as es:
                    rot = Rot(range(8))
                    rotp = Rot([0, 1])
                    rots_d = [Rot([2, 3, 4]), Rot([5, 6, 7])]
                    PB, rPB = SB(es, "PB", [128, 11, 512])
                    S.D(PB[:, 0:2, :].rearrange("p a b -> p (a b)"), I["rwkv_w0"][l:l + 1].rearrange("o a b -> o (a b)").partition_broadcast(128), writes=[rPB])
                    S.D(PB[:, 2:4, :].rearrange("p a b -> p (a b)"), I["rwkv_a0"][l:l + 1].rearrange("o a b -> o (a b)").partition_broadcast(128), writes=[rPB])
                    S.D(PB[:, 4, :], I["rwkv_k_k"][l:l + 1, :].partition_broadcast(128), writes=[rPB])
                    S.D(PB[:, 5, :], I["rwkv_k_a"][l:l + 1, :].partition_broadcast(128), writes=[rPB])
                    S.D(PB[:, 7:9, :].rearrange("p a b -> p (a b)"), I["rwkv_r_k"][l:l + 1].rearrange("o a b -> o (a b)").partition_broadcast(128), writes=[rPB])
                    S.D(PB[:, 9, :], I["rwkv_ln_w"][l:l + 1, :].partition_broadcast(128), writes=[rPB])
                    S.D(PB[:, 10, :], I["rwkv_ln_b"][l:l + 1, :].partition_broadcast(128), writes=[rPB])
                    ts("pool", PB[:, 6, :], PB[:, 5, :], -1.0, 1.0, ALU.mult, ALU.add, [rPB], [rPB])
                    LW, rLW = SB(es, "LW", [128, 3, 512])
                    S.D(LW[:, 0, :], I["rwkv_w2"][l], writes=[rLW])
                    S.D(LW[:, 1, :], I["rwkv_a2"][l], writes=[rLW])
                    S.D(LW[:, 2, :], I["rwkv_g2"][l], writes=[rLW])
                    TRI, rTRI = SB(es, "TRI", [128, 4, 128])
                    CHI, rCHI = SB(es, "CHI", [128, 2])
                    MK, rMK = SB(es, "MK", [128, 4, 128])
                    S.D(TRI[:], C["tri"], writes=[rTRI])
                    S.D(CHI[:], C["chunkind"], writes=[rCHI])
                    S.D(MK[:], C["mask4"], writes=[rMK])
                    ucbufs = [SB(es, "ucv_", [128, 1920]) for d_ in range(2)]
                    ucmap = {}
                    F5 = {n: SB(es, "r_" + n, [128, 512]) for n in ("sig", "a", "kk", "kdir", "kka", "E1", "E2", "E3", "E4", "tA", "tB", "tC")}
                    B5 = {n: SB(es, "rb_" + n, [128, 512], BF16) for n in ("RTm", "BTm", "KTm")}
                    lin, rlin = SB(es, "lin", [128, 384])
                    twT, rtwT = SB(es, "twT", [128, 3, 128])
                    sm, rsm = SB(es, "smallr", [128, 64])
                    HB = []
                    for hb_i in range(2):
                        hbd = {"XAR": SB(es, "XAR", [64, 8, 2, 128], BF16), "XB": SB(es, "XB", [64, 8, 128], BF16),
                               "XK": SB(es, "XK", [64, 8, 128], BF16), "RP": SB(es, "RP", [64, 8, 2, 128], BF16),
                               "PL": SB(es, "PL", [64, 8, 2]), "AL": SB(es, "hAL", [128, 512], BF16), "VB": SB(es, "hVB", [128, 512], BF16),
                               "BL": SB(es, "hBL", [128, 512], BF16), "KL": SB(es, "hKL", [128, 512], BF16)}
                        S.I("pool", "memset", [], [hbd["RP"][1]], ap=hbd["RP"][0][:], constant=0.0)
                        HB.append(hbd)
                    SI = []
                    for d_ in range(2):
                        SI.append({"AK2": SB(es, "AK2", [128, 8, 2, 128], BF16),
                                   "GT": [SB(es, "GT%d" % i, [128, 8, 3, 128], BF16) for i in range(2)],
                                   "FF": [SB(es, "FF%d" % i, [128, 8, 128], BF16) for i in range(2)],
                                   "WT": SB(es, "WT", [64, 8, 128], BF16), "X1": SB(es, "X1", [128, 8, 64], BF16),
                                   "Ub": SB(es, "Ub", [128, 8, 64], BF16),
                                   "SF": [SB(es, "SF%d" % i, [64, 8, 64]) for i in range(3)],
                                   "SBF": [SB(es, "SBF%d" % i, [64, 8, 64], BF16) for i in range(3)]})
                    stmp_sh = SB(es, "stmp", [64, 8, 64])
                    ysb_sh = SB(es, "ysb", [128, 512])

                    def F(n):
                        return F5[n]

                    def rwkv_prep(dr, ti, hb):
                        XAR, rXAR = hb["XAR"]
                        XB, rXB = hb["XB"]
                        XK, rXK = hb["XK"]
                        RP, rRP = hb["RP"]
                        PL, rPL = hb["PL"]
                        AL, rAL = hb["AL"]
                        VB, rVB = hb["VB"]
                        BL, rBL = hb["BL"]
                        KL, rKL = hb["KL"]
                        rows = slice(ti * 128, (ti + 1) * 128)
                        first_of_seq = ti in (0, 2)
                        last_of_seq = ti in (1, 33)
                        need_y = not (last and ti < 2)
                        uc, ruc = ucmap[(dr, ti)]
                        r_ = uc[:, 0:512]
                        k_ = uc[:, 512:1024]
                        v_ = uc[:, 1024:1536]
                        yield
                        act(lin[:, 0:128], uc[:, 1536:1664], AF.Tanh, [ruc], [rlin])
                        cp("pool", lin[:, 128:256], uc[:, 1664:1792], [ruc], [rlin])
                        nl = 2
                        if dr == 0:
                            act(lin[:, 256:384], uc[:, 1792:1920], AF.Sigmoid, [ruc], [rlin])
                            nl = 3
                        pt, rpt = rotp.next()
                        for i3 in range(nl):
                            tp(pt[:, i3 * 128:(i3 + 1) * 128], lin[:, i3 * 128:(i3 + 1) * 128], ident[:], [rlin, rident], [rpt])
                        cp("act", twT[:, 0:nl, :].rearrange("p a b -> p (a b)"), pt[:, 0:nl * 128], [rpt], [rtwT])
                        ds_ = slice(dr * 64, (dr + 1) * 64)
                        pz, rpz = rotp.next()
                        mm(pz[:], twT[ds_, 0, :], LW[ds_, 0, :], True, True, [rtwT, rLW], [rpz])
                        pa, rpa = rotp.next()
                        mm(pa[:], twT[ds_, 1, :], LW[ds_, 1, :], True, True, [rtwT, rLW], [rpa])
                        sig, rsig = F("sig")
                        a_, ra_ = F("a")
                        tA, rtA = F("tA")
                        tB, rtB = F("tB")
                        tC, rtC = F("tC")
                        tt("dve", tA[:], pz[:], PB[:, dr, :], ALU.add, [rpz, rPB], [rtA])
                        act(sig[:], tA[:], AF.Sigmoid, [rtA], [rsig])
                        tt("dve", tB[:], pa[:], PB[:, 2 + dr, :], ALU.add, [rpa, rPB], [rtB])
                        act(a_[:], tB[:], AF.Sigmoid, [rtB], [ra_])
                        yield
                        t_incl = dr
                        t_before = 2 if dr == 0 else 3
                        t_after = 3 if dr == 0 else 2
                        E1, rE1 = F("E1")
                        E2, rE2 = F("E2")
                        E3, rE3 = F("E3")
                        E4, rE4 = F("E4")
                        pc, rpc = rotp.next()
                        mm(pc[:], TRI[:, t_incl, :], sig[:], True, True, [rTRI, rsig], [rpc])
                        act(E1[:], pc[:], AF.Exp, [rpc], [rE1])
                        act(E3[:], pc[:], AF.Exp, [rpc], [rE3], scale=-1.0)
                        pc, rpc = rotp.next()
                        mm(pc[:], TRI[:, t_before, :], sig[:], True, True, [rTRI, rsig], [rpc])
                        act(E2[:], pc[:], AF.Exp, [rpc], [rE2])
                        pc, rpc = rotp.next()
                        mm(pc[:], TRI[:, t_after, :], sig[:], True, True, [rTRI, rsig], [rpc])
                        act(E4[:], pc[:], AF.Exp, [rpc], [rE4])
                        ppl, rppl = rotp.next()
                        for h in range(8):
                            mm(ppl[0:64, h * 2:(h + 1) * 2], sig[:, h * 64:(h + 1) * 64], CHI[:], True, True, [rsig, rCHI], [rppl])
                        act(PL[:].rearrange("p h c -> p (h c)"), ppl[0:64, 0:16], AF.Exp, [rppl], [rPL])
                        yield
                        kk, rkk = F("kk")
                        kdir, rkdir = F("kdir")
                        kka, rkka = F("kka")
                        tt("pool", tB[:], k_, PB[:, 4, :], ALU.mult, [ruc, rPB, rtB], [rtB])
                        tt("pool", tC[:], tB[:], tB[:], ALU.mult, [rtB, rtC], [rtC])
                        red(sm[:, 0:8], tC[:].rearrange("p (h d) -> p h d", d=64), ALU.add, [rtC], [rsm])
                        act(sm[:, 8:16], sm[:, 0:8], AF.Sqrt, [rsm], [rsm], scale=1.0)
                        ts("dve", sm[:, 8:16], sm[:, 8:16], 1e-12, None, ALU.max, None, [rsm], [rsm])
                        S.I("dve", "reciprocal", [rsm], [rsm], out=sm[:, 8:16], in_=sm[:, 8:16])
                        tt("dve", kk[:].rearrange("p (h d) -> p h d", d=64), tB[:].rearrange("p (h d) -> p h d", d=64),
                           sm[:, 8:16].unsqueeze(2).to_broadcast([128, 8, 64]), ALU.mult, [rtB, rsm], [rkk])
                        tt("pool", tA[:], a_[:], PB[:, 5, :], ALU.mult, [ra_, rPB, rtA], [rtA])
                        tt("pool", tA[:], tA[:], PB[:, 6, :], ALU.add, [rtA, rPB], [rtA])
                        tt("pool", kdir[:], k_, tA[:], ALU.mult, [ruc, rtA], [rkdir])
                        tt("pool", kka[:], kk[:], a_[:], ALU.mult, [rkk, ra_], [rkka])
                        yield
                        RTm, rRTm = B5["RTm"]
                        BTm, rBTm = B5["BTm"]
                        KTm, rKTm = B5["KTm"]
                        tt("dve", RTm[:], r_, E1[:], ALU.mult, [ruc, rE1], [rRTm])
                        stt(AL[:], kk[:], -1.0, E2[:], ALU.mult, ALU.mult, [rkk, rE2], [rAL])
                        tt("pool", BTm[:], kka[:], E3[:], ALU.mult, [rkka, rE3], [rBTm])
                        tt("pool", KTm[:], kdir[:], E3[:], ALU.mult, [rkdir, rE3], [rKTm])
                        tt("dve", BL[:], kka[:], E4[:], ALU.mult, [rkka, rE4], [rBL])
                        tt("pool", KL[:], kdir[:], E4[:], ALU.mult, [rkdir, rE4], [rKL])
                        cp("act", VB[:], v_, [ruc], [rVB])
                        yield
                        for (src, rsrc, kind) in ((AL, rAL, 0), (RTm, rRTm, 1), (BTm, rBTm, 2), (KTm, rKTm, 3)):
                            pt, rpt = rotp.next()
                            pv = bfv(pt)
                            for h in range(8):
                                tp(pv[0:64, h * 128:(h + 1) * 128], src[:, h * 64:(h + 1) * 64], identb[:], [rsrc, ridentb], [rpt])
                            pv3 = pv[0:64, :].rearrange("p (h t) -> p h t", h=8)
                            if kind == 0:
                                cp("act", XAR[:, :, 0, :], pv3, [rpt], [rXAR])
                            elif kind == 1:
                                cp("act", XAR[:, :, 1, :], pv3, [rpt], [rXAR])
                                cp("act", RP[:, :, 0, 0:64], pv3[:, :, 0:64], [rpt], [rRP])
                                cp("act", RP[:, :, 1, 64:128], pv3[:, :, 64:128], [rpt], [rRP])
                            elif kind == 2:
                                cp("act", XB[:], pv3, [rpt], [rXB])
                            else:
                                cp("act", XK[:], pv3, [rpt], [rXK])
                            yield
                        yield
                        if dr == 0:
                            g1, rg1 = F("E1")
                            g2_, rg2_ = F("E2")
                            aO, raO = F("E3")
                            po_, rpo_ = rotp.next()
                            mm(po_[:], twT[64:128, 1, :], LW[64:128, 1, :], True, True, [rtwT, rLW], [rpo_])
                            tt("dve", tC[:], po_[:], PB[:, 3, :], ALU.add, [rpo_, rPB, rtC], [rtC])
                            act(aO[:], tC[:], AF.Sigmoid, [rtC], [raO])
                            pg, rpg = rotp.next()
                            mm(pg[:], twT[:, 2, :], LW[:, 2, :], True, True, [rtwT, rLW], [rpg])
                            tt("dve", tA[:], r_, kdir[:], ALU.mult, [ruc, rkdir, rtA], [rtA])
                            tt("pool", tA[:], tA[:], PB[:, 7, :], ALU.mult, [rtA, rPB], [rtA])
                            red(sm[:, 16:24], tA[:].rearrange("p (h d) -> p h d", d=64), ALU.add, [rtA], [rsm])
                            tt("pool", tC[:], aO[:], PB[:, 5, :], ALU.mult, [raO, rPB, rtC], [rtC])
                            tt("pool", tC[:], tC[:], PB[:, 6, :], ALU.add, [rtC, rPB], [rtC])
                            tt("pool", tC[:], tC[:], k_, ALU.mult, [rtC, ruc], [rtC])
                            tt("dve", tC[:], tC[:], r_, ALU.mult, [rtC, ruc], [rtC])
                            tt("pool", tC[:], tC[:], PB[:, 8, :], ALU.mult, [rtC, rPB], [rtC])
                            red(sm[:, 24:32], tC[:].rearrange("p (h d) -> p h d", d=64), ALU.add, [rtC], [rsm])
                            tt("dve", sm[:, 16:24], sm[:, 16:24], sm[:, 24:32], ALU.add, [rsm], [rsm])
                            tt("dve", tA[:].rearrange("p (h d) -> p h d", d=64), v_.rearrange("p (h d) -> p h d", d=64),
                               sm[:, 16:24].unsqueeze(2).to_broadcast([128, 8, 64]), ALU.mult, [ruc, rsm, rtA], [rtA])
                            tt("pool", tA[:], tA[:], PB[:, 10, :], ALU.add, [rtA, rPB], [rtA])
                            tt("dve", g2_[:], tA[:], pg[:], ALU.mult, [rtA, rpg], [rg2_])
                            tt("dve", g1[:], PB[:, 9, :], pg[:], ALU.mult, [rPB, rpg], [rg1])
                            S.D(G1D[rows, :], g1[:], reads=[rg1], writes=[rG[ti]])
                            S.D(G2D[rows, :], g2_[:], reads=[rg2_], writes=[rG[ti]])
                    def rwkv_solve(dr, ti, sbox, hb):
                        XAR, rXAR = hb["XAR"]
                        XB, rXB = hb["XB"]
                        XK, rXK = hb["XK"]
                        RP, rRP = hb["RP"]
                        PL, rPL = hb["PL"]
                        AL, rAL = hb["AL"]
                        VB, rVB = hb["VB"]
                        BL, rBL = hb["BL"]
                        KL, rKL = hb["KL"]
                        rows = slice(ti * 128, (ti + 1) * 128)
                        need_y = not (last and ti < 2)
                        si = sbox[0]
                        sd_ = SI[dr]
                        rots = rots_d[dr]
                        AK2, rAK2 = sd_["AK2"]
                        GT = sd_["GT"]
                        FF = sd_["FF"]
                        WT, rWT = sd_["WT"]
                        X1, rX1 = sd_["X1"]
                        Ub, rUb = sd_["Ub"]
                        stmp, rstmp = stmp_sh
                        SF = sd_["SF"]
                        SBF = sd_["SBF"]
                        m_s = 0 if dr == 0 else 2
                        m_i = 1 if dr == 0 else 3
                        m_n = 2 if dr == 0 else 0
                        G0, rG0 = GT[0]
                        mk2 = MK[:, 2 * dr:2 * dr + 2, :].unsqueeze(1).to_broadcast([128, 2, 2, 128])
                        for hp in range(4):
                            hs = slice(2 * hp, 2 * hp + 2)
                            for (lhs, rl, dst, rdst) in ((XB, rXB, G0[:, hs, 0:3:2, :], rG0), (XK, rXK, AK2[:, hs, :, :], rAK2)):
                                pq, rpq = rots.next()
                                for hh in range(2):
                                    h = 2 * hp + hh
                                    mm(pq[:, hh * 256:(hh + 1) * 256], lhs[:, h, :], XAR[:, h, :, :].rearrange("p a t -> p (a t)"), True, True, [rl, rXAR], [rpq])
                                pq4 = pq[:].rearrange("p (h a t) -> p h a t", h=2, a=2)
                                tt("dve", dst, pq4, mk2, ALU.mult, [rpq, rMK], [rdst])
                        yield
                        F0, rF0 = FF[0]
                        for hq in range(2):
                            pq, rpq = rots.next()
                            for hh in range(4):
                                h = 4 * hq + hh
                                mm(pq[:, hh * 128:(hh + 1) * 128], XAR[:, h, 0, :], XB[:, h, :], True, True, [rXAR, rXB], [rpq])
                            tt("dve", F0[:, 4 * hq:4 * hq + 4, :], pq[:].rearrange("p (h t) -> p h t", h=4), MK[:, m_n, :].unsqueeze(1).to_broadcast([128, 4, 128]), ALU.mult, [rpq, rMK], [rF0])
                        cp("pool", G0[:, :, 1, :], identb[:].unsqueeze(1).to_broadcast([128, 8, 128]), [ridentb], [rG0])
                        yield
                        for s_ in range(6):
                            cur, nxt = s_ % 2, 1 - s_ % 2
                            Gc, rGc = GT[cur]
                            Gn, rGn = GT[nxt]
                            Fc, rFc = FF[cur]
                            Fn, rFn = FF[nxt]
                            if s_ < 5:
                                for hq in range(2):
                                    pq, rpq = rots.next()
                                    for hh in range(4):
                                        h = 4 * hq + hh
                                        mm(pq[:, hh * 128:(hh + 1) * 128], Gc[:, h, 0, :], Fc[:, h, :], True, True, [rGc, rFc], [rpq])
                                    cp("act", Fn[:, 4 * hq:4 * hq + 4, :], pq[:].rearrange("p (h t) -> p h t", h=4), [rpq], [rFn])
                                for hp in range(4):
                                    hs = slice(2 * hp, 2 * hp + 2)
                                    pq, rpq = rots.next()
                                    for hh in range(2):
                                        h = 2 * hp + hh
                                        mm(pq[:, hh * 256:(hh + 1) * 256], Fc[:, h, :], Gc[:, h, 0:2, :].rearrange("p a t -> p (a t)"), True, True, [rFc, rGc], [rpq])
                                    pq4 = pq[:].rearrange("p (h a t) -> p h a t", h=2, a=2)
                                    cp("act", Gn[:, hs, 0, :], pq4[:, :, 0, :], [rpq], [rGn])
                                    tt("dve", Gn[:, hs, 1, :], pq4[:, :, 1, :], Gc[:, hs, 1, :], ALU.add, [rpq, rGc], [rGn])
                            else:
                                for hq in range(2):
                                    pq, rpq = rots.next()
                                    for hh in range(4):
                                        h = 4 * hq + hh
                                        mm(pq[:, hh * 128:(hh + 1) * 128], Fc[:, h, :], Gc[:, h, 1, :], True, True, [rFc, rGc], [rpq])
                                    tt("dve", Gn[:, 4 * hq:4 * hq + 4, 1, :], pq[:].rearrange("p (h t) -> p h t", h=4), Gc[:, 4 * hq:4 * hq + 4, 1, :], ALU.add, [rpq, rGc], [rGn])
                            yield
                        TTt, rTT = GT[0]
                        for hq in range(2):
                            pq, rpq = rots.next()
                            for hh in range(4):
                                h = 4 * hq + hh
                                mm(pq[0:64, hh * 128:(hh + 1) * 128], AL[:, h * 64:(h + 1) * 64], TTt[:, h, 1, :], True, True, [rAL, rTT], [rpq])
                            cp("act", WT[:, 4 * hq:4 * hq + 4, :], pq[0:64, :].rearrange("p (h t) -> p h t", h=4), [rpq], [rWT])
                        pq, rpq = rots.next()
                        for h in range(8):
                            mm(pq[:, h * 64:(h + 1) * 64], AK2[:, h, 0, :], VB[:, h * 64:(h + 1) * 64], True, True, [rAK2, rVB], [rpq])
                        cp("dve", X1[:].rearrange("p h v -> p (h v)"), pq[:], [rpq], [rX1])
                        yield
                        sidx = {}
                        for c in ((0, 1) if dr == 0 else (1, 0)):
                            crow = slice(c * 64, (c + 1) * 64)
                            M = 64 if c == 0 else 128
                            S0f, rS0f = SF[si]
                            S0b, rS0b = SBF[si]
                            sn = (si + 1) % 3
                            S1f, rS1f = SF[sn]
                            S1b, rS1b = SBF[sn]
                            sidx[c] = si
                            pu, rpu = rots.next()
                            for h in range(8):
                                mm(pu[0:M, h * 64:(h + 1) * 64], WT[:, h, 0:M], S0b[:, h, :], True, False, [rWT, rS0b], [rpu])
                                if c == 0:
                                    mm(pu[0:M, h * 64:(h + 1) * 64], TTt[crow, h, 1, 0:M], X1[crow, h, :], False, True, [rTT, rX1], [rpu])
                                else:
                                    mm(pu[0:M, h * 64:(h + 1) * 64], TTt[:, h, 1, 0:M], X1[:, h, :], False, True, [rTT, rX1], [rpu])
                            cp("act", Ub[crow, :, :].rearrange("p h v -> p (h v)"), pu[crow, :], [rpu], [rUb])
                            psn, rpsn = rots.next()
                            for h in range(8):
                                mm(psn[0:64, h * 64:(h + 1) * 64], BL[crow, h * 64:(h + 1) * 64], Ub[crow, h, :], True, False, [rBL, rUb], [rpsn])
                                mm(psn[0:64, h * 64:(h + 1) * 64], KL[crow, h * 64:(h + 1) * 64], VB[crow, h * 64:(h + 1) * 64], False, True, [rKL, rVB], [rpsn])
                            tt("pool", stmp[:], S0f[:], PL[:, :, c:c + 1].to_broadcast([64, 8, 64]), ALU.mult, [rS0f, rPL], [rstmp])
                            tt("dve", S1f[:].rearrange("p h v -> p (h v)"), stmp[:].rearrange("p h v -> p (h v)"), psn[0:64, :], ALU.add, [rstmp, rpsn], [rS1f])
                            cp("act", S1b[:], S1f[:], [rS1f], [rS1b])
                            si = sn
                            yield
                        if need_y:
                            py, rpy = rots.next()
                            for h in range(8):
                                hc = slice(h * 64, (h + 1) * 64)
                                mm(py[:, hc], RP[:, h, 0, :], SBF[sidx[0]][0][:, h, :], True, False, [rRP, SBF[sidx[0]][1]], [rpy])
                                mm(py[:, hc], RP[:, h, 1, :], SBF[sidx[1]][0][:, h, :], False, False, [rRP, SBF[sidx[1]][1]], [rpy])
                                mm(py[:, hc], GT[0][0][:, h, 2, :], Ub[:, h, :], False, False, [GT[0][1], rUb], [rpy])
                                mm(py[:, hc], AK2[:, h, 1, :], VB[:, hc], False, True, [rAK2, rVB], [rpy])
                            ysb, rysb = ysb_sh
                            cp("act", ysb[:], py[:], [rpy], [rysb])
                            S.D(YD[dr][rows, :], ysb[:], reads=[rysb], writes=[rY[dr][ti]])
                        sbox[0] = si

                    def chain(dr):
                        order = list(range(NT)) if dr == 0 else [1, 0] + list(range(NT - 1, 1, -1))
                        if KT_LIMIT is not None:
                            order = [t_ for t_ in order if t_ < KT_LIMIT - 1]
                        SF0, SBF0 = SI[dr]["SF"][0], SI[dr]["SBF"][0]
                        S.I("pool", "memset", [], [SF0[1]], ap=SF0[0][:], constant=0.0)
                        S.I("pool", "memset", [], [SBF0[1]], ap=SBF0[0][:], constant=0.0)
                        sbox = [0]

                        def uc_load(t_):
                            ub_, rub_ = ucbufs[dr]
                            S.D(ub_[:], UC[t_ * 128:(t_ + 1) * 128, :], reads=[rUC[t_]], writes=[rub_], queue="pool")
                            ucmap[(dr, t_)] = (ub_, rub_)
                        uc_load(order[0])
                        for oi, ti in enumerate(order):
                            while prep_lock[0] is not None and prep_lock[0] != dr:
                                yield
                            prep_lock[0] = dr
                            for _ in rwkv_prep(dr, ti, HB[dr]):
                                yield
                            prep_lock[0] = None
                            if oi + 1 < len(order):
                                uc_load(order[oi + 1])
                            for _ in rwkv_solve(dr, ti, sbox, HB[dr]):
                                yield

                    prep_lock = [None]
                    chains = [chain(0), chain(1)]
                    alive = [True, True]
                    while any(alive):
                        for ci_ in range(2):
                            if alive[ci_]:
                                try:
                                    next(chains[ci_])
                                except StopIteration:
                                    alive[ci_] = False
                S.barrier()
                if stop_after == "E":
                    break
                ftiles = list(range(2, NT)) if last else list(range(NT))
                if KT_LIMIT is not None:
                    ftiles = [t_ for t_ in ftiles if t_ < KT_LIMIT - 1]
                with ExitStack() as es:
                    rot = Rot(range(8))
                    Wout, rWout = SB(es, "Wout", [128, 8, 1024], BF16)
                    S.D(Wout[:], I["w_out"][l].rearrange("(k p) n -> p k n", p=128), writes=[rWout], queue="pool")
                    oaring = Ring(es, "oTa", [128, 4, 128], BF16, 3)
                    orring = Ring(es, "oTr", [128, 4, 128], BF16, 2)
                    yring = Ring(es, "yio", [128, 4, 512], F32, 3)
                    h1ring = Ring(es, "h1t", [128, 1024], F32, 2)
                    hring = Ring(es, "htf", [128, 1024], F32, 3)
                    obring = Ring(es, "obf2", [128, 512], BF16, 2)
                    o_, ro_ = SB(es, "o_sum", [128, 512])
                    oc_, roc_ = SB(es, "o_cen", [128, 512])
                    sq_, rsq_ = SB(es, "o_sq", [128, 512])
                    gs, rgs = SB(es, "gnst", [128, 32])
                    pend_f = []
                    for ti in ftiles:
                        rows = slice(ti * 128, (ti + 1) * 128)
                        tcols = slice(ti * 128, (ti + 1) * 128)
                        which = 1 if ti < 2 else 0
                        oTa, roTa = oaring.next()
                        S.D(oTa[:], OT[0:512, tcols].rearrange("(c p) t -> p c t", p=128), reads=[], writes=[roTa])
                        yio, ryio = yring.next()
                        S.D(yio[:, 0, :], YD[0][rows, :], reads=[rY[0][ti]], writes=[ryio])
                        S.D(yio[:, 1, :], YD[1][rows, :], reads=[rY[1][ti]], writes=[ryio])
                        S.D(yio[:, 2, :], G1D[rows, :], reads=[rG[ti]], writes=[ryio])
                        S.D(yio[:, 3, :], G2D[rows, :], reads=[rG[ti]], writes=[ryio])
                        ht, rht = hring.next()
                        S.D(ht[:], hsrc(l, ti), reads=[rH[ti]], writes=[rht])
                        v3 = lambda t_: t_[:].rearrange("p (h d) -> p h d", d=64)
                        tt("pool", o_[:], yio[:, 0, :], yio[:, 1, :], ALU.add, [ryio], [ro_])
                        red(gs[:, 0:8], v3(o_), ALU.add, [ro_], [rgs])
                        ts("dve", gs[:, 8:16], gs[:, 0:8], 1.0 / 64.0, None, ALU.mult, None, [rgs], [rgs])
                        tt("dve", v3(oc_), v3(o_), gs[:, 8:16].unsqueeze(2).to_broadcast([128, 8, 64]), ALU.subtract, [ro_, rgs], [roc_])
                        tt("pool", sq_[:], oc_[:], oc_[:], ALU.mult, [roc_], [rsq_])
                        red(gs[:, 16:24], v3(sq_), ALU.add, [rsq_], [rgs])
                        act(gs[:, 24:32], gs[:, 16:24], AF.Sqrt, [rgs], [rgs], bias=64e-5, scale=1.0 / 64.0)
                        S.I("dve", "reciprocal", [rgs], [rgs], out=gs[:, 24:32], in_=gs[:, 24:32])
                        tt("dve", v3(sq_), v3(oc_), gs[:, 24:32].unsqueeze(2).to_broadcast([128, 8, 64]), ALU.mult, [roc_, rgs, rsq_], [rsq_])
                        tt("pool", sq_[:], sq_[:], yio[:, 2, :], ALU.mult, [rsq_, ryio], [rsq_])
                        ob, rob = obring.next()
                        tt("pool", ob[:], sq_[:], yio[:, 3, :], ALU.add, [rsq_, ryio], [rob])
                        pt, rpt = rot.next()
                        pv = bfv(pt)
                        for k in range(4):
                            tp(pv[:, k * 128:(k + 1) * 128], ob[:, k * 128:(k + 1) * 128], identb[:], [rob, ridentb], [rpt])
                        oTr, roTr = orring.next()
                        cp("act", oTr[:].rearrange("p c t -> p (c t)"), pv[:, 0:512], [rpt], [roTr])
                        def stage2(oTa=oTa, roTa=roTa, oTr=oTr, roTr=roTr, ht=ht, rht=rht, rows=rows, which=which, ti=ti):
                            h1, rh1 = h1ring.next()
                            for half in range(2):
                                hc = slice(half * 512, (half + 1) * 512)
                                pq, rpq = rot.next()
                                for k in range(8):
                                    lhs = oTa[:, k, :] if k < 4 else oTr[:, k - 4, :]
                                    mm(pq[:], lhs, Wout[:, k, hc], k == 0, k == 7, [roTa, roTr, rWout], [rpq])
                                tt("dve", h1[:, hc], pq[:], GM[:, 0, which, hc], ALU.mult, [rpq, rGM], [rh1])
                            tt("pool", h1[:], h1[:], ht[:], ALU.add, [rh1, rht], [rh1])
                            S.D(H1[rows, :], h1[:], reads=[rh1], writes=[rH1[ti]], queue="pool")
                        for fn_ in pend_f:
                            fn_()
                        pend_f[:] = [stage2]
                    for fn_ in pend_f:
                        fn_()
                S.barrier()
                if stop_after == "F1":
                    break
                with ExitStack() as es:
                    rot = Rot(range(8))
                    GTL = 8
                    WR, rWR = SB(es, "WR", [128, 8, 16])
                    S.D(WR[:], I["w_router"].rearrange("(k p) e -> p k e", p=128), writes=[rWR])
                    RB, rRB = SB(es, "RB", [128, 16])
                    S.D(RB[:], I["router_bias"].partition_broadcast(128), writes=[rRB])
                    if last:
                        FNG, rFNG = SB(es, "FNG", [128, 1024])
                        S.D(FNG[:], I["final_norm_g"].partition_broadcast(128), writes=[rFNG])
                    wring = Ring(es, "wexp", [128, 8, 1024], BF16, 5)
                    FT, rFT = SB(es, "FT", [128, 8, GTL * 128], BF16)
                    YA, rYA = SB(es, "YA", [128, GTL, 1024])
                    GATE, rGATE = SB(es, "GATE", [128, GTL, 16])
                    ACTT, rACTT = SB(es, "ACTT", [128, 8, 512], BF16)
                    rings = {"junk": Ring(es, "junk2", [128, 1024], F32, 2), "st": Ring(es, "st2", [128, 4], F32, 2),
                             "xnf": Ring(es, "xnf", [128, 1024], F32, 2)}
                    xfring = Ring(es, "xf", [128, 8, 128], F32, 2)
                    hring = Ring(es, "ht2", [128, 1024], F32, 2)
                    sgring = Ring(es, "sg", [128, 512], F32, 2)
                    rtring = Ring(es, "rt", [128, 160], F32, 2)
                    groups = [ftiles[i:i + GTL] for i in range(0, len(ftiles), GTL)]
                    for grp in groups:
                        for gi, ti in enumerate(grp):
                            rows = slice(ti * 128, (ti + 1) * 128)
                            which = 1 if ti < 2 else 0
                            ht, rht = hring.next()
                            xf, rxf = xfring.next()
                            rt, rrt = rtring.next()
                            S.D(ht[:], H1[rows, :], reads=[rH1[ti]], writes=[rht])
                            norm_mod_T(rings, ht, rht, which, 2, FT[:, :, gi * 128:(gi + 1) * 128], rFT, rot, fp32=True, xf=xf, rxf=rxf)
                            pl_, rpl_ = rot.next()
                            for k in range(8):
                                mm(pl_[:, 0:16], xf[:, k, :], WR[:, k, :], k == 0, k == 7, [rxf, rWR], [rpl_])
                            sc_ = rt[:, 0:16]
                            bi = rt[:, 16:32]
                            bi3 = bi.rearrange("p (g e) -> p g e", g=4)
                            act(sc_, pl_[:, 0:16], AF.Sigmoid, [rpl_], [rrt])
                            tt("dve", bi, sc_, RB[:], ALU.add, [rrt, rRB], [rrt])
                            red(rt[:, 32:36], bi3, ALU.max, [rrt], [rrt])
                            eq3 = rt[:, 48:64].rearrange("p (g e) -> p g e", g=4)
                            tt("dve", eq3, bi3, rt[:, 32:36].unsqueeze(2).to_broadcast([128, 4, 4]), ALU.is_equal, [rrt], [rrt])
                            b23 = rt[:, 64:80].rearrange("p (g e) -> p g e", g=4)
                            stt(b23, eq3, -10.0, bi3, ALU.mult, ALU.add, [rrt], [rrt])
                            red(rt[:, 36:40], b23, ALU.max, [rrt], [rrt])
                            tt("dve", rt[:, 40:44], rt[:, 32:36], rt[:, 36:40], ALU.add, [rrt], [rrt])
                            red(rt[:, 44:45], rt[:, 40:44], ALU.max, [rrt], [rrt])
                            ts("dve", rt[:, 80:84], rt[:, 40:44], rt[:, 44:45], None, ALU.is_equal, None, [rrt], [rrt])
                            ts("dve", rt[:, 80:84], rt[:, 80:84], -1.0, 10.0, ALU.add, ALU.mult, [rrt], [rrt])
                            mk3 = rt[:, 96:112].rearrange("p (g e) -> p g e", g=4)
                            tt("dve", mk3, bi3, rt[:, 80:84].unsqueeze(2).to_broadcast([128, 4, 4]), ALU.add, [rrt], [rrt])
                            red(rt[:, 45:46], rt[:, 96:112], ALU.max, [rrt], [rrt])
                            ts("dve", rt[:, 112:128], rt[:, 96:112], rt[:, 45:46], None, ALU.is_equal, None, [rrt], [rrt])
                            stt(rt[:, 128:144], rt[:, 112:128], -10.0, rt[:, 96:112], ALU.mult, ALU.add, [rrt], [rrt])
                            red(rt[:, 46:47], rt[:, 128:144], ALU.max, [rrt], [rrt])
                            ts("dve", rt[:, 144:160], rt[:, 128:144], rt[:, 46:47], None, ALU.is_equal, None, [rrt], [rrt])
                            tt("dve", rt[:, 112:128], rt[:, 112:128], rt[:, 144:160], ALU.add, [rrt], [rrt])
                            tt("dve", rt[:, 112:128], rt[:, 112:128], sc_, ALU.mult, [rrt], [rrt])
                            red(rt[:, 47:48], rt[:, 112:128], ALU.add, [rrt], [rrt])
                            S.I("dve", "reciprocal", [rrt], [rrt], out=rt[:, 47:48], in_=rt[:, 47:48])
                            ts("dve", GATE[:, gi, :], rt[:, 112:128], rt[:, 47:48], None, ALU.mult, None, [rrt], [rGATE])
                        ntg = len(grp)
                        chunks = [(c0, min(4, ntg - c0)) for c0 in range(0, ntg, 4)]
                        for e_ in range(16):
                            Wg, rWg = wring.next()
                            S.D(Wg[:], I["e_gate"][l, e_].rearrange("(k p) n -> p k n", p=128), writes=[rWg], queue="pool")
                            Wu, rWu = wring.next()
                            S.D(Wu[:], I["e_up"][l, e_].rearrange("(k p) n -> p k n", p=128), writes=[rWu], queue="pool")
                            Wd, rWd = wring.next()
                            S.D(Wd[:], I["e_down"][l, e_].rearrange("(k p) n -> p k n", p=128), writes=[rWd], queue="pool")
                            for (c0, nct) in chunks:
                                ncol = nct * 128
                                tk = slice(c0 * 128, c0 * 128 + ncol)
                                for fc in range(8):
                                    fs = slice(fc * 128, (fc + 1) * 128)
                                    pg, rpg = rot.next()
                                    for k in range(8):
                                        mm(pg[:, 0:ncol], Wg[:, k, fs], FT[:, k, tk], k == 0, k == 7, [rWg, rFT], [rpg])
                                    pu_, rpu_ = rot.next()
                                    for k in range(8):
                                        mm(pu_[:, 0:ncol], Wu[:, k, fs], FT[:, k, tk], k == 0, k == 7, [rWu, rFT], [rpu_])
                                    sg, rsg = sgring.next()
                                    act(sg[:, 0:ncol], pg[:, 0:ncol], AF.Silu, [rpg], [rsg])
                                    tt("dve", ACTT[:, fc, 0:ncol], sg[:, 0:ncol], pu_[:, 0:ncol], ALU.mult, [rsg, rpu_], [rACTT])
                                for t4 in range(nct):
                                    gi = c0 + t4
                                    for half in range(2):
                                        hc = slice(half * 512, (half + 1) * 512)
                                        py, rpy = rot.next()
                                        for fc in range(8):
                                            mm(py[:], ACTT[:, fc, t4 * 128:(t4 + 1) * 128], Wd[:, fc, hc], fc == 0, fc == 7, [rACTT, rWd], [rpy])
                                        if e_ == 0:
                                            ts("dve", YA[:, gi, hc], py[:], GATE[:, gi, e_:e_ + 1], None, ALU.mult, None, [rpy, rGATE], [rYA])
                                        else:
                                            stt(YA[:, gi, hc], py[:], GATE[:, gi, e_:e_ + 1], YA[:, gi, hc], ALU.mult, ALU.add, [rpy, rGATE, rYA], [rYA])
                        for gi, ti in enumerate(grp):
                            rows = slice(ti * 128, (ti + 1) * 128)
                            which = 1 if ti < 2 else 0
                            ht, rht = hring.next()
                            S.D(ht[:], H1[rows, :], reads=[rH1[ti]], writes=[rht])
                            tt("dve", YA[:, gi, :], YA[:, gi, :], GM[:, 1, which, :], ALU.mult, [rYA, rGM], [rYA])
                            tt("dve", ht[:], ht[:], YA[:, gi, :], ALU.add, [rht, rYA], [rht])
                            if not last:
                                S.D(H[rows, :], ht[:], reads=[rht], writes=[rH[ti]], queue="pool")
                            else:
                                junk, rjunk = rings["junk"].next()
                                st, rst = rings["st"].next()
                                act(junk[:], ht[:], AF.Square, [rht], [rjunk, rst], scale=1.0 / 32.0, accum_out=st[:, 0:1])
                                act(st[:, 1:2], st[:, 0:1], AF.Sqrt, [rst], [rst], bias=1e-6, scale=1.0)
                                S.I("dve", "reciprocal", [rst], [rst], out=st[:, 2:3], in_=st[:, 1:2])
                                ts("dve", junk[:], ht[:], st[:, 2:3], None, ALU.mult, None, [rht, rst, rjunk], [rjunk])
                                tt("pool", junk[:], junk[:], FNG[:], ALU.mult, [rjunk, rFNG], [rjunk])
                                S.D(OUT[(ti - 2) * 128:(ti - 1) * 128, :], junk[:], reads=[rjunk], writes=[rOUT], queue="pool")
                S.barrier()
                if stop_after == "F2":
                    break
        S.finish([rOUT, rOT, rU] + rH + rH1 + rUC + rY[0] + rY[1] + rG + ([rdd] if dbg else []))
        S.replay()
    return nc


_NC_CACHE = {}


def make_in_maps(inputs, batches):
    consts = host_consts()
    in_maps = []
    for b in batches:
        m = {}
        for k, shp in IN_SHAPES.items():
            a = np.asarray(inputs[k], dtype=np.float32)
            if k in ("x", "ctx"):
                a = a[b]
            elif k == "c":
                a = a[b:b + 1]
            m[k] = np.ascontiguousarray(a.reshape(shp))
        for k, v in consts.items():
            m["k_" + k] = v
        in_maps.append(m)
    return in_maps


def kernel(**inputs):
    if "nc" not in _NC_CACHE:
        _NC_CACHE["nc"] = build()
    nc = _NC_CACHE["nc"]
    in_maps = make_in_maps(inputs, range(8))
    res = run_bass_kernel_spmd(nc, in_maps, core_ids=list(range(8)))
    return np.stack([r["out"] for r in res.results], axis=0).astype(np.float32)
```

```python
import numpy as np
import concourse.bass as bass
import concourse.mybir as mybir
from concourse.bass_utils import run_bass_kernel_spmd
from contextlib import ExitStack

F32 = mybir.dt.float32
BF16 = mybir.dt.bfloat16
AF = mybir.ActivationFunctionType
ALU = mybir.AluOpType
AX = mybir.AxisListType

SAME_ENGINE_SYNC = True
N_DMA_SEMS = 8
NT = 34
T = 4352
DEPTH = 2
EXPD = 0.6065306597126334
KT_LIMIT = None
DBG_CUT = None
SKIP_D = False
E_CUT = None
GTL_OVERRIDE = None
E_VAR = 0


class Res:
    __slots__ = ("name", "lw", "rd")

    def __init__(self, name):
        self.name = name
        self.lw = None
        self.rd = []


class Ins:
    __slots__ = ("eng", "idx", "fn", "deps", "clk", "need_inc", "semkey", "val", "is_dma")


class Sched:
    def __init__(self, nc):
        self.nc = nc
        self.q = {e: [] for e in ("pe", "act", "dve", "pool", "sp")}
        self.known = {e: {} for e in self.q}
        self.cnt = {}
        self.last_on_sem = {}
        self.dma_rr = {"sp": 0, "pool": 0, "act": 0}
        self.final = []

    def _issue(self, eng, fn, reads, writes, semkey, is_dma):
        ins = Ins()
        ins.eng = eng
        ins.fn = fn
        ins.is_dma = is_dma
        ins.need_inc = is_dma
        ins.semkey = semkey
        self.cnt[semkey] = self.cnt.get(semkey, 0) + 1
        ins.idx = self.cnt[semkey]
        deps = []
        for r in reads:
            if r.lw is not None:
                deps.append(r.lw)
        for w in writes:
            if w.lw is not None:
                deps.append(w.lw)
            deps.extend(w.rd)
        if is_dma:
            prev = self.last_on_sem.get(semkey)
            if prev is not None:
                deps.append(prev)
            self.last_on_sem[semkey] = ins
        known = self.known[eng]
        need = {}
        for d in deps:
            if d is ins:
                continue
            if (not d.is_dma) and d.eng == eng:
                if eng == "pe" or not SAME_ENGINE_SYNC:
                    continue
            if known.get(d.semkey, 0) >= d.idx:
                continue
            if need.get(d.semkey) is None or need[d.semkey].idx < d.idx:
                need[d.semkey] = d
        ins.deps = list(need.values())
        for d in ins.deps:
            d.need_inc = True
            for k, v in d.clk.items():
                if known.get(k, 0) < v:
                    known[k] = v
            if known.get(d.semkey, 0) < d.idx:
                known[d.semkey] = d.idx
        ins.clk = dict(known)
        for r in reads:
            r.rd.append(ins)
        for w in writes:
            w.lw = ins
            w.rd = []
        self.q[eng].append(ins)
        return ins

    def op(self, eng, fn, reads=(), writes=()):
        return self._issue(eng, fn, reads, writes, eng, False)

    def dma(self, fn, reads=(), writes=(), queue="sp"):
        k = self.dma_rr[queue]
        self.dma_rr[queue] = (k + 1) % N_DMA_SEMS
        return self._issue(queue, fn, reads, writes, "dma_%s_%d" % (queue, k), True)

    def I(self, eng, meth, reads=(), writes=(), **kw):
        return self.op(eng, lambda e: getattr(e, meth)(**kw), reads, writes)

    def D(self, out, in_, reads=(), writes=(), queue="sp"):
        return self.dma(lambda e: e.dma_start(out=out, in_=in_), reads, writes, queue)

    def barrier(self):
        lasts = []
        for eng, lst in self.q.items():
            for ins in reversed(lst):
                if ins.fn is not None and not ins.is_dma:
                    lasts.append(ins)
                    break
        lasts.extend(self.last_on_sem.values())
        for eng in self.q:
            ins = Ins()
            ins.eng = eng
            ins.fn = None
            ins.is_dma = False
            ins.need_inc = False
            ins.semkey = eng
            ins.idx = self.cnt.get(eng, 0)
            known = self.known[eng]
            ins.deps = []
            for d in lasts:
                if (not d.is_dma) and d.eng == eng:
                    continue
                if known.get(d.semkey, 0) >= d.idx:
                    continue
                ins.deps.append(d)
                d.need_inc = True
            for d in ins.deps:
                for k, v in d.clk.items():
                    if known.get(k, 0) < v:
                        known[k] = v
                if known.get(d.semkey, 0) < d.idx:
                    known[d.semkey] = d.idx
            ins.clk = dict(known)
            self.q[eng].append(ins)

    def finish(self, out_res):
        self.final = [r.lw for r in out_res if r.lw is not None]

    def replay(self):
        nc = self.nc
        semkeys = list(self.cnt.keys())
        vals = {k: 0 for k in semkeys}
        for f in self.final:
            f.need_inc = True
        for eng, lst in self.q.items():
            for ins in lst:
                if ins.need_inc:
                    vals[ins.semkey] += 16 if ins.is_dma else 1
                    ins.val = vals[ins.semkey]
        with ExitStack() as es:
            es.enter_context(nc.allow_non_contiguous_dma(reason="small strided parameter loads"))
            sems = {k: es.enter_context(nc.semaphore("s_" + k)) for k in semkeys}
            block = es.enter_context(nc.Block())
            engmap = {"pe": block.tensor, "act": block.scalar, "dve": block.vector,
                      "pool": block.gpsimd, "sp": block.sync}
            for eng, lst in self.q.items():
                final = self.final if eng == "sp" else []

                def body(e, lst=lst, final=final):
                    for ins in lst:
                        for d in ins.deps:
                            e.wait_ge(sems[d.semkey], d.val)
                        if ins.fn is None:
                            continue
                        r = ins.fn(e)
                        if ins.need_inc:
                            r.then_inc(sems[ins.semkey], 16 if ins.is_dma else 1)
                    for f in final:
                        e.wait_ge(sems[f.semkey], f.val)
                engmap[eng](body)


def host_consts():
    c = {}
    c["ident"] = np.eye(128, dtype=np.float32)
    n = 4096
    row = np.repeat(np.arange(n // 64, dtype=np.float32), 64)
    col = np.tile(np.arange(64, dtype=np.float32), n // 64)
    inv = (np.float32(10000.0) ** (-np.arange(16, dtype=np.float32) / np.float32(16))).astype(np.float32)
    ang = np.concatenate([row[:, None] * inv, col[:, None] * inv], axis=-1).astype(np.float32)
    c["rope"] = np.concatenate([np.cos(ang), np.sin(ang)], axis=-1).astype(np.float32)
    i = np.arange(128)[:, None]
    j = np.arange(128)[None, :]
    wm = np.zeros((128, 384), np.float32)
    wm[:, 0:128] = np.where(j < i, -30000.0, 0.0)
    wm[:, 256:384] = np.where(j > i, -30000.0, 0.0)
    c["winmask"] = wm
    same = (i // 64) == (j // 64)
    tri = np.zeros((4, 128, 128), np.float32)
    tri[0] = np.where(same & (i <= j), -EXPD, 0.0)
    tri[1] = np.where(same & (i >= j), -EXPD, 0.0)
    tri[2] = np.where(same & (i < j), -EXPD, 0.0)
    tri[3] = np.where(same & (i > j), -EXPD, 0.0)
    c["tri"] = np.ascontiguousarray(tri.transpose(1, 0, 2))
    ind = np.zeros((128, 2), np.float32)
    ind[0:64, 0] = -EXPD
    ind[64:128, 1] = -EXPD
    c["chunkind"] = ind
    mk = np.zeros((128, 4, 128), np.float32)
    mk[:, 0] = same & (i < j)
    mk[:, 1] = same & (i <= j)
    mk[:, 2] = same & (i > j)
    mk[:, 3] = same & (i >= j)
    c["mask4"] = mk
    sel = np.zeros((2, 2, 128), np.float32)
    sel[0, 0] = 1.0
    sel[1, 1] = 1.0
    c["sel"] = sel
    return c


CONST_SHAPES = {"ident": [128, 128], "rope": [4096, 64], "winmask": [128, 384], "tri": [128, 4, 128],
                "chunkind": [128, 2], "mask4": [128, 4, 128], "sel": [2, 2, 128]}

IN_SHAPES = {
    "x": [4096, 1024], "c": [1, 1024], "ctx": [256, 1024], "c_ctx": [1, 1024],
    "w_mod": [2, 1024, 6144], "b_mod": [2, 6144], "norm_mix_g": [2, 1024], "norm_ffn_g": [2, 1024],
    "w_in": [2, 1024, 2944], "q_norm_g": [2, 64], "k_norm_g": [2, 64], "sink_logit": [2, 4],
    "rwkv_conv": [2, 3, 1920], "rwkv_w0": [2, 2, 512], "rwkv_w2": [2, 128, 512], "rwkv_a0": [2, 2, 512],
    "rwkv_a2": [2, 128, 512], "rwkv_g2": [2, 128, 512], "rwkv_k_k": [2, 512], "rwkv_k_a": [2, 512],
    "rwkv_r_k": [2, 2, 512], "rwkv_ln_w": [2, 512], "rwkv_ln_b": [2, 512], "w_out": [2, 1024, 1024],
    "w_router": [1024, 16], "router_bias": [1, 16], "e_gate": [2, 16, 1024, 1024],
    "e_up": [2, 16, 1024, 1024], "e_down": [2, 16, 1024, 1024], "final_norm_g": [1, 1024],
}


def build(stop_after=None, dbg=False):
    nc = bass.Bass("TRN2", target_bir_lowering=False)
    I = {k: nc.dram_tensor(k, s, F32, kind="ExternalInput").ap() for k, s in IN_SHAPES.items()}
    C = {k: nc.dram_tensor("k_" + k, s, F32, kind="ExternalInput").ap() for k, s in CONST_SHAPES.items()}
    OUT = nc.dram_tensor("out", [4096, 1024], F32, kind="ExternalOutput").ap()
    skind = "ExternalOutput" if dbg else "Internal"
    H = nc.dram_tensor("H", [T, 1024], F32, kind=skind).ap()
    H1 = nc.dram_tensor("H1", [T, 1024], F32, kind=skind).ap()
    U = nc.dram_tensor("U", [T, 1920], F32, kind=skind).ap()
    UC = nc.dram_tensor("UC", [T, 1920], F32, kind=skind).ap()
    YD = [nc.dram_tensor("YF", [T, 512], F32, kind=skind).ap(), nc.dram_tensor("YB", [T, 512], F32, kind=skind).ap()]
    G1D = nc.dram_tensor("G1", [T, 512], F32, kind=skind).ap()
    G2D = nc.dram_tensor("G2", [T, 512], F32, kind=skind).ap()
    OT = nc.dram_tensor("OT", [1024, T], BF16, kind=skind).ap()
    S = Sched(nc)
    rH = [Res("H%d" % i) for i in range(NT)]
    rH1 = [Res("H1%d" % i) for i in range(NT)]
    rU = Res("U")
    rUC = [Res("UC%d" % i) for i in range(NT)]
    rY = [[Res("Y%d_%d" % (d, i)) for i in range(NT)] for d in range(2)]
    rG = [Res("G%d" % i) for i in range(NT)]
    rOT = Res("OT")
    rOUT = Res("OUT")

    def mm(out, lhsT, rhs, start, stop, reads, writes):
        S.I("pe", "matmul", reads, writes, out=out, lhsT=lhsT, rhs=rhs, start=start, stop=stop)

    def tp(out, in_, idn, reads, writes):
        S.I("pe", "transpose", reads, writes, out=out, in_=in_, identity=idn)

    def tt(eng, out, in0, in1, op, reads, writes):
        S.I(eng, "tensor_tensor", reads, writes, out=out, in0=in0, in1=in1, op=op)

    def ts(eng, out, in0, s1, s2, op0, op1, reads, writes):
        if op1 is None:
            S.I(eng, "tensor_scalar", reads, writes, out=out, in0=in0, scalar1=s1, scalar2=None, op0=op0)
        else:
            S.I(eng, "tensor_scalar", reads, writes, out=out, in0=in0, scalar1=s1, scalar2=s2, op0=op0, op1=op1)

    def stt(out, in0, scalar, in1, op0, op1, reads, writes):
        S.I("dve", "scalar_tensor_tensor", reads, writes, out=out, in0=in0, scalar=scalar, in1=in1, op0=op0, op1=op1)

    def act(out, in_, func, reads, writes, **kw):
        S.I("act", "activation", reads, writes, out=out, in_=in_, func=func, **kw)

    def cp(eng, out, in_, reads, writes):
        if eng == "act":
            S.I("act", "copy", reads, writes, out=out, in_=in_)
        else:
            S.I(eng, "tensor_copy", reads, writes, out=out, in_=in_)

    def red(out, in_, op, reads, writes, axis=AX.X, **kw):
        S.I("dve", "tensor_reduce", reads, writes, out=out, in_=in_, axis=axis, op=op, **kw)

    def hsrc(l, ti):
        if l == 0:
            return I["ctx"][ti * 128:(ti + 1) * 128, :] if ti < 2 else I["x"][(ti - 2) * 128:(ti - 1) * 128, :]
        return H[ti * 128:(ti + 1) * 128, :]

    with ExitStack() as top:
        uid = [0]

        def SB(es, name, shape, dt=F32):
            uid[0] += 1
            name = "%s_%d" % (name, uid[0])
            return es.enter_context(nc.sbuf_tensor(name, shape, dt)), Res(name)

        class Ring:
            def __init__(self, es, name, shape, dt, n):
                self.items = [SB(es, "%s%d" % (name, i), shape, dt) for i in range(n)]
                self.i = 0

            def next(self):
                it = self.items[self.i]
                self.i = (self.i + 1) % len(self.items)
                return it

        PS = [(top.enter_context(nc.psum_tensor("ps%d" % i, [128, 512], F32)), Res("ps%d" % i)) for i in range(8)]

        class Rot:
            def __init__(self, ids):
                self.ids = list(ids)
                self.i = 0

            def next(self):
                p = PS[self.ids[self.i]]
                self.i = (self.i + 1) % len(self.ids)
                return p

        def bfv(pt):
            return pt[:].bitcast(BF16)

        ident, rident = SB(top, "ident", [128, 128])
        identb, ridentb = SB(top, "identb", [128, 128], BF16)
        S.D(ident[:], C["ident"], writes=[rident])
        S.D(identb[:], C["ident"], writes=[ridentb], queue="pool")
        sel, rsel = SB(top, "sel", [2, 2, 128])
        S.D(sel[:], C["sel"], writes=[rsel])
        ones, rones = SB(top, "ones", [128, 128])
        S.I("pool", "memset", [], [rones], ap=ones[:], constant=1.0)
        cc, rcc = SB(top, "cc", [128, 8, 2])
        ccr, rccr = SB(top, "ccr", [128, 8, 2])
        S.D(ccr[:, :, 0], I["c"].rearrange("o (k p) -> p (o k)", p=128), writes=[rccr])
        S.D(ccr[:, :, 1], I["c_ctx"].rearrange("o (k p) -> p (o k)", p=128), writes=[rccr])
        act(cc[:], ccr[:], AF.Silu, [rccr], [rcc])

        for l in range(DEPTH):
            last = (l == DEPTH - 1)
            with ExitStack() as LS:
                FM, rFM = SB(LS, "FM", [128, 4, 8, 2])
                GM, rGM = SB(LS, "GM", [128, 2, 2, 1024])
                with ExitStack() as es:
                    rot = Rot(range(8))
                    R, rR = SB(es, "Rmod", [2, 6144])
                    bm, rbm = SB(es, "bm", [2, 6144])
                    g2, rg2 = SB(es, "g2p", [2, 2, 1024])
                    AB, rAB = SB(es, "AB", [2, 4, 1024])
                    S.D(bm[:], I["b_mod"][l:l + 1, :].partition_broadcast(2), writes=[rbm])
                    S.D(g2[:, 0, :], I["norm_mix_g"][l:l + 1, :].partition_broadcast(2), writes=[rg2])
                    S.D(g2[:, 1, :], I["norm_ffn_g"][l:l + 1, :].partition_broadcast(2), writes=[rg2])
                    wring = Ring(es, "wm", [128, 8, 512], F32, 2)
                    for j in range(12):
                        wm, rwm = wring.next()
                        S.D(wm[:], I["w_mod"][l, :, j * 512:(j + 1) * 512].rearrange("(k p) n -> p k n", p=128), writes=[rwm])
                        pt, rpt = rot.next()
                        for k in range(8):
                            mm(pt[0:2, :], cc[:, k, :], wm[:, k, :], k == 0, k == 7, [rcc, rwm], [rpt])
                        tt("dve", R[:, j * 512:(j + 1) * 512], pt[0:2, :], bm[:, j * 512:(j + 1) * 512], ALU.add, [rpt, rbm], [rR])
                    stt(AB[:, 0, :], R[:, 1024:2048], 1.0, g2[:, 0, :], ALU.add, ALU.mult, [rR, rg2], [rAB])
                    cp("dve", AB[:, 1, :], R[:, 0:1024], [rR], [rAB])
                    stt(AB[:, 2, :], R[:, 4096:5120], 1.0, g2[:, 1, :], ALU.add, ALU.mult, [rR, rg2], [rAB])
                    cp("dve", AB[:, 3, :], R[:, 3072:4096], [rR], [rAB])
                    pt, rpt = rot.next()
                    for v in range(4):
                        for k in range(8):
                            o = (v * 8 + k) * 2
                            tp(pt[:, o:o + 2], AB[:, v, k * 128:(k + 1) * 128], ident[0:2, 0:2], [rAB, rident], [rpt])
                    cp("dve", FM[:].rearrange("p a b c -> p (a b c)"), pt[:, 0:64], [rpt], [rFM])
                    for gi, off in ((0, 2048), (1, 5120)):
                        for w in range(2):
                            for hh in range(2):
                                pt, rpt = rot.next()
                                mm(pt[:], sel[:, w, :], R[:, off + hh * 512:off + (hh + 1) * 512], True, True, [rsel, rR], [rpt])
                                cp("act", GM[:, gi, w, hh * 512:(hh + 1) * 512], pt[:], [rpt], [rGM])
                S.barrier()
                if dbg:
                    DFM = nc.dram_tensor("DFM%d" % l, [128, 64], F32, kind="ExternalOutput").ap()
                    DGM = nc.dram_tensor("DGM%d" % l, [128, 4096], F32, kind="ExternalOutput").ap()
                    rdd = Res("dd")
                    S.D(DFM, FM[:].rearrange("p a b c -> p (a b c)"), reads=[rFM], writes=[rdd])
                    S.D(DGM, GM[:].rearrange("p a b c -> p (a b c)"), reads=[rGM], writes=[rdd])
                if stop_after == "A":
                    break

                def norm_mod_T(rg, ht, rht, which, vsel, xmT, rxmT, pbanks, fp32=False, xf=None, rxf=None):
                    junk, rjunk = rg["junk"].next()
                    st, rst = rg["st"].next()
                    act(junk[:], ht[:], AF.Square, [rht], [rjunk, rst], scale=1.0 / 32.0, accum_out=st[:, 0:1])
                    act(st[:, 1:2], st[:, 0:1], AF.Sqrt, [rst], [rst], bias=1e-6, scale=1.0)
                    S.I("dve", "reciprocal", [rst], [rst], out=st[:, 2:3], in_=st[:, 1:2])
                    if not fp32:
                        xn, rxn = rg["xn"].next()
                        ts("dve", xn[:], ht[:], st[:, 2:3], None, ALU.mult, None, [rht, rst], [rxn])
                        pt, rpt = pbanks.next()
                        pv = bfv(pt)
                        for k in range(8):
                            tp(pv[:, k * 128:(k + 1) * 128], xn[:, k * 128:(k + 1) * 128], identb[:], [rxn, ridentb], [rpt])
                        for k in range(8):
                            act(xmT[:, k, :], pv[:, k * 128:(k + 1) * 128], AF.Identity, [rpt, rFM], [rxmT],
                                scale=FM[:, vsel, k, which:which + 1], bias=FM[:, vsel + 1, k, which:which + 1])
                    else:
                        xn, rxn = rg["xnf"].next()
                        ts("dve", xn[:], ht[:], st[:, 2:3], None, ALU.mult, None, [rht, rst], [rxn])
                        for hh in range(2):
                            pt, rpt = pbanks.next()
                            for k in range(4):
                                kk = hh * 4 + k
                                tp(pt[:, k * 128:(k + 1) * 128], xn[:, kk * 128:(kk + 1) * 128], ident[:], [rxn, rident], [rpt])
                            for k in range(4):
                                kk = hh * 4 + k
                                act(xf[:, kk, :], pt[:, k * 128:(k + 1) * 128], AF.Identity, [rpt, rFM], [rxf],
                                    scale=FM[:, vsel, kk, which:which + 1], bias=FM[:, vsel + 1, kk, which:which + 1])
                        cp("dve", xmT, xf[:], [rxf], [rxmT])

                with ExitStack() as AS:
                    QT, rQT = SB(AS, "QT", [128, 2, T], BF16)
                    KT, rKT = SB(AS, "KT", [128, T], BF16)
                    VG, rVG = SB(AS, "VG", [128, NT, 2, 65], BF16)
                    QWT, rQWT = SB(AS, "QWT", [128, 2, T], BF16)
                    KWT, rKWT = SB(AS, "KWT", [128, T], BF16)
                    VW, rVW = SB(AS, "VW", [128, NT, 2, 64], BF16)
                    S.I("pool", "memset", [], [rVG], ap=VG[:], constant=1.0)
                    with ExitStack() as es:
                        Win, rWin = SB(es, "Win", [128, 8, 2944], BF16)
                        for k in range(8):
                            wsrc = I["w_in"][l, k * 128:(k + 1) * 128, :]
                            for qo in (0, 512):
                                for j in range(2):
                                    S.D(Win[:, k, qo + j * 128:qo + (j + 1) * 128].rearrange("p (g d) -> p g d", g=2),
                                        wsrc[:, qo:qo + 256].rearrange("p (g j d) -> p g j d", g=2, j=2)[:, :, j, :], writes=[rWin], queue="pool")
                            S.D(Win[:, k, 256:512], wsrc[:, 256:512], writes=[rWin], queue="pool")
                            S.D(Win[:, k, 768:2944], wsrc[:, 768:2944], writes=[rWin], queue="pool")
                        G6, rG6 = SB(es, "G6", [128, 6, 64])
                        S.D(G6[:, 0, :], I["q_norm_g"][l:l + 1, :].partition_broadcast(128), writes=[rG6])
                        S.D(G6[:, 4, :], I["k_norm_g"][l:l + 1, :].partition_broadcast(128), writes=[rG6])
                        for hh in (1, 2, 3):
                            cp("pool", G6[:, hh, :], G6[:, 0, :], [rG6], [rG6])
                        cp("pool", G6[:, 5, :], G6[:, 4, :], [rG6], [rG6])
                        rings = {"junk": Ring(es, "junk", [128, 1024], F32, 1), "st": Ring(es, "st", [128, 4], F32, 2),
                                 "xn": Ring(es, "xn", [128, 1024], BF16, 2)}
                        hring = Ring(es, "ht", [128, 1024], F32, 2)
                        xring = Ring(es, "xmT", [128, 8, 128], BF16, 2)
                        uaring = Ring(es, "ua", [128, 1024], F32, 2)
                        urring = Ring(es, "ur", [128, 1920], F32, 2)
                        csring = Ring(es, "cs", [128, 64], F32, 2)
                        qrring = Ring(es, "qr", [128, 12, 64], BF16, 2)
                        tmpring = Ring(es, "tmpb", [128, 6, 64], F32, 4)
                        sring = Ring(es, "ssb", [128, 16], F32, 2)
                        rotT = Rot([0, 1])
                        rotU = Rot([2, 3, 4, 5])
                        rotQ = Rot([6, 7])
                        pend_qk = []
                        b_tiles = list(range(NT if KT_LIMIT is None else KT_LIMIT))
                        xm_of = {}

                        def b_norm(ti_):
                            which_ = 1 if ti_ < 2 else 0
                            ht, rht = hring.next()
                            S.D(ht[:], hsrc(l, ti_), reads=[rH[ti_]], writes=[rht])
                            xm_, rxm_ = xring.next()
                            norm_mod_T(rings, ht, rht, which_, 0, xm_, rxm_, rotT)
                            xm_of[ti_] = (xm_, rxm_)
                        b_norm(b_tiles[0])
                        for ti in b_tiles:
                            which = 1 if ti < 2 else 0
                            if ti + 1 <= b_tiles[-1]:
                                b_norm(ti + 1)
                            xmT, rxmT = xm_of.pop(ti)
                            ua, rua = uaring.next()
                            ur, rur = urring.next()
                            for cch in range(6):
                                c0 = cch * 512
                                cw = min(512, 2944 - c0)
                                pt, rpt = rotU.next()
                                for k in range(8):
                                    mm(pt[:, 0:cw], xmT[:, k, :], Win[:, k, c0:c0 + cw], k == 0, k == 7, [rxmT, rWin], [rpt])
                                if cch < 2:
                                    dst, rdst = ua[:, c0:c0 + 512], rua
                                else:
                                    dst, rdst = ur[:, c0 - 1024:c0 - 1024 + cw], rur
                                cp("act" if cch % 2 == 0 else "dve", dst, pt[:, 0:cw], [rpt], [rdst])
                            S.D(U[ti * 128:(ti + 1) * 128, :], ur[:], reads=[rur], writes=[Res("u")])
                            if DBG_CUT == 2:
                                continue
                            qr, rqr = qrring.next()
                            ss, rss = sring.next()
                            t1, rt1 = tmpring.next()
                            t2, rt2 = tmpring.next()
                            uq = ua[:, 0:384].rearrange("p (h d) -> p h d", d=64)
                            uw = ua[:, 512:896].rearrange("p (h d) -> p h d", d=64)
                            tt("pool", t1[:], uq, uq, ALU.mult, [rua], [rt1])
                            red(ss[:, 0:6], t1[:], ALU.add, [rt1], [rss])
                            act(ss[:, 6:12], ss[:, 0:6], AF.Sqrt, [rss], [rss], bias=1e-6, scale=1.0 / 64.0)
                            S.I("dve", "reciprocal", [rss], [rss], out=ss[:, 6:12], in_=ss[:, 6:12])
                            tt("dve", t1[:], uq, ss[:, 6:12].unsqueeze(2).to_broadcast([128, 6, 64]), ALU.mult, [rua, rss], [rt1])
                            if ti < 2:
                                tt("pool", qr[:, 0:6, :], t1[:], G6[:], ALU.mult, [rt1, rG6], [rqr])
                                cp("pool", qr[:, 6:12, :], uw, [rua], [rqr])
                            else:
                                cs, rcs = csring.next()
                                S.D(cs[:], C["rope"][(ti - 2) * 128:(ti - 1) * 128, :], writes=[rcs])
                                tt("pool", t2[:], t1[:], G6[:], ALU.mult, [rt1, rG6], [rt2])
                                for (src, rsrc, o6, eng2) in ((t2[:], rt2, 0, "dve"), (uw, rua, 6, "pool")):
                                    sv = src.rearrange("p h (i two) -> p h i two", two=2)
                                    x1, x2 = sv[:, :, :, 0], sv[:, :, :, 1]
                                    cosb = cs[:, 0:32].unsqueeze(1).to_broadcast([128, 6, 32])
                                    sinb = cs[:, 32:64].unsqueeze(1).to_broadcast([128, 6, 32])
                                    ta, rta = tmpring.next()
                                    tav = ta[:].rearrange("p h (k i) -> p h k i", k=2)
                                    ov = qr[:, o6:o6 + 6, :].rearrange("p h (i two) -> p h i two", two=2)
                                    tt(eng2, tav[:, :, 0, :], x1, cosb, ALU.mult, [rsrc, rcs], [rta])
                                    tt(eng2, tav[:, :, 1, :], x2, sinb, ALU.mult, [rsrc, rcs], [rta])
                                    tt(eng2, ov[:, :, :, 0], tav[:, :, 0, :], tav[:, :, 1, :], ALU.subtract, [rta], [rqr])
                                    tt(eng2, tav[:, :, 0, :], x1, sinb, ALU.mult, [rsrc, rcs, rta], [rta])
                                    tt(eng2, tav[:, :, 1, :], x2, cosb, ALU.mult, [rsrc, rcs, rta], [rta])
                                    tt(eng2, ov[:, :, :, 1], tav[:, :, 0, :], tav[:, :, 1, :], ALU.add, [rta], [rqr])
                            cp("pool", VG[:, ti, :, 0:64], ua[:, 384:512].rearrange("p (g d) -> p g d", d=64), [rua], [rVG])
                            cp("pool", VW[:, ti, :, :], ua[:, 896:1024].rearrange("p (g d) -> p g d", d=64), [rua], [rVW])
                            def qk_T(qr=qr, rqr=rqr, ti=ti):
                                pt, rpt = rotQ.next()
                                pv = bfv(pt)
                                for o6, tq in ((0, 0), (6, 3)):
                                    for j in range(2):
                                        tp(pv[:, (tq + j) * 128:(tq + j + 1) * 128], qr[:, o6 + 2 * j:o6 + 2 * j + 2, :].rearrange("p h d -> p (h d)"), identb[:], [rqr, ridentb], [rpt])
                                    tp(pv[:, (tq + 2) * 128:(tq + 3) * 128], qr[:, o6 + 4:o6 + 6, :].rearrange("p h d -> p (h d)"), identb[:], [rqr, ridentb], [rpt])
                                tcols = slice(ti * 128, (ti + 1) * 128)
                                cp("act", QT[:, :, tcols], pv[:, 0:256].rearrange("p (j t) -> p j t", j=2), [rpt], [rQT])
                                cp("act", KT[:, tcols], pv[:, 256:384], [rpt], [rKT])
                                cp("act", QWT[:, :, tcols], pv[:, 384:640].rearrange("p (j t) -> p j t", j=2), [rpt], [rQWT])
                                cp("act", KWT[:, tcols], pv[:, 640:768], [rpt], [rKWT])
                            for fn_ in pend_qk:
                                fn_()
                            pend_qk[:] = [qk_T]
                        for fn_ in pend_qk:
                            fn_()
                    S.barrier()
                    if stop_after == "B":
                        if dbg and KT_LIMIT != 0 and DBG_CUT is None:
                            ncl = (NT if KT_LIMIT is None else KT_LIMIT) * 128
                            DQT = nc.dram_tensor("DQT", [128, 2, T], BF16, kind="ExternalOutput").ap()
                            DKT = nc.dram_tensor("DKT", [128, T], BF16, kind="ExternalOutput").ap()
                            DQW = nc.dram_tensor("DQW", [128, 2, T], BF16, kind="ExternalOutput").ap()
                            DKW = nc.dram_tensor("DKW", [128, T], BF16, kind="ExternalOutput").ap()
                            rdd = Res("dd2")
                            S.D(DQT[:, :, 0:ncl], QT[:, :, 0:ncl], reads=[rQT], writes=[rdd])
                            S.D(DKT[:, 0:ncl], KT[:, 0:ncl], reads=[rKT], writes=[rdd])
                            S.D(DQW[:, :, 0:ncl], QWT[:, :, 0:ncl], reads=[rQWT], writes=[rdd])
                            S.D(DKW[:, 0:ncl], KWT[:, 0:ncl], reads=[rKWT], writes=[rdd])
                        break
                    with ExitStack() as es:
                      CW, rCW = SB(es, "CW", [128, 3, 1920])
                      S.D(CW[:].rearrange("p a b -> p (a b)"), I["rwkv_conv"][l:l + 1].rearrange("o a b -> o (a b)").partition_broadcast(128), writes=[rCW])
                      e0_ust = [SB(es, "ust%d_" % i, [128, 1920]) for i in range(3)]
                      e0_ucr = Ring(es, "ucv0_", [128, 1920], F32, 2)
                      e0_tiles = list(range(NT if KT_LIMIT is None else KT_LIMIT - 1))
                      e0_state = {}

                      def e0_stage1(ti):
                          first_of_seq = ti in (0, 2)
                          last_of_seq = ti in (1, 33)
                          uc, ruc = e0_ucr.next()
                          e0_state[ti] = (uc, ruc)
                          for jj, off in ((0, -1), (1, 0), (2, 1)):
                              u_, ru_ = e0_ust[jj]
                              if off == -1 and first_of_seq:
                                  S.I("pool", "memset", [], [ru_], ap=u_[:], constant=0.0)
                                  S.D(u_[1:128, :], U[ti * 128:ti * 128 + 127, :], reads=[], writes=[ru_])
                              elif off == 1 and last_of_seq:
                                  S.I("pool", "memset", [], [ru_], ap=u_[:], constant=0.0)
                                  S.D(u_[0:127, :], U[ti * 128 + 1:ti * 128 + 128, :], reads=[], writes=[ru_])
                              else:
                                  S.D(u_[:], U[ti * 128 + off:ti * 128 + off + 128, :], reads=[], writes=[ru_])
                          tt("pool", uc[:], e0_ust[0][0][:], CW[:, 0, :], ALU.mult, [e0_ust[0][1], rCW], [ruc])
                          tt("pool", e0_ust[2][0][:], e0_ust[2][0][:], CW[:, 2, :], ALU.mult, [e0_ust[2][1], rCW], [e0_ust[2][1]])

                      def e0_stage2(ti):
                          uc, ruc = e0_state.pop(ti)
                          tt("dve", e0_ust[1][0][:], e0_ust[1][0][:], CW[:, 1, :], ALU.mult, [e0_ust[1][1], rCW], [e0_ust[1][1]])
                          tt("dve", uc[:], uc[:], e0_ust[1][0][:], ALU.add, [ruc, e0_ust[1][1]], [ruc])
                          tt("dve", uc[:], uc[:], e0_ust[2][0][:], ALU.add, [ruc, e0_ust[2][1]], [ruc])
                          S.D(UC[ti * 128:(ti + 1) * 128, :], uc[:], reads=[ruc], writes=[rUC[ti]])
                      e0_sched = []
                      for t_ in e0_tiles:
                          e0_sched.append((e0_stage1, t_))
                          e0_sched.append((e0_stage2, t_))

                      def e0_step():
                          if e0_sched:
                              fn_, t_ = e0_sched.pop(0)
                              fn_(t_)
                      if SKIP_D:
                        while e0_sched:
                            e0_step()
                        zt, rzt = SB(es, "zt", [128, 4, 128], BF16)
                        S.I("pool", "memset", [], [rzt], ap=zt[:], constant=0.0)
                        for ti_ in range(KT_LIMIT):
                            S.D(OT[0:512, ti_ * 128:(ti_ + 1) * 128].rearrange("(c p) t -> p c t", p=128), zt[:], reads=[rzt], writes=[rOT])
                      if not SKIP_D:
                          nb, rnb = SB(es, "negb", [128, 4])
                          gk, rgk = SB(es, "gk", [1, 132])
                          SK, rSK = SB(es, "SK", [128, 4])
                          WM, rWM = SB(es, "WM", [128, 384])
                          S.D(WM[:], C["winmask"], writes=[rWM])
                          S.D(SK[:], I["sink_logit"][l:l + 1, :].partition_broadcast(128), writes=[rSK])
                          S.D(gk[:, 0:64], I["q_norm_g"][l:l + 1, :], writes=[rgk])
                          S.D(gk[:, 64:128], I["k_norm_g"][l:l + 1, :], writes=[rgk])
                          red(gk[:, 128:130], gk[:, 0:128].rearrange("p (a b) -> p a b", a=2), ALU.max, [rgk], [rgk], apply_absolute_value=True)
                          stt(gk[:, 130:131], gk[:, 128:129], -8.0, gk[:, 129:130], ALU.mult, ALU.mult, [rgk], [rgk])
                          pt, rpt = PS[7]
                          mm(pt[:, 0:1], ones[0:1, :], gk[0:1, 130:131], True, True, [rones, rgk], [rpt])
                          cp("dve", nb[:, 0:1], pt[:, 0:1], [rpt], [rnb])
                          pring = Ring(es, "pT", [128, 512], BF16, 5)
                          oring = Ring(es, "osb", [64, 512], F32, 2)
                          obring = Ring(es, "obf", [64, 512], BF16, 2)
                          rdring = Ring(es, "rden", [128, 512], F32, 2)
                          rotO = Rot([0, 1])
                          rotS = Rot([2, 3, 4, 5])
                          rotB = Rot([6, 7])
                          blocks = [(256 + qb * 256, list(range(NT))) for qb in range(16)]
                          if not last:
                              blocks = [(0, [0, 1])] + blocks
                          for g in range(2):
                              gs = slice(g * 64, (g + 1) * 64)
                              for (q0, kcs) in blocks:
                                  e0_step()
                                  po, rpo = rotO.next()
                                  pend = []
                                  for ci, kc in enumerate(kcs):
                                      ps_, rps_ = rotS.next()
                                      mm(ps_[:], KT[gs, kc * 128:(kc + 1) * 128], QT[gs, :, q0:q0 + 256], True, True, [rKT, rQT], [rps_])
                                      pT, rpT = pring.next()
                                      act(pT[:], ps_[:], AF.Exp, [rps_, rnb], [rpT], scale=0.125, bias=nb[:, 0:1])
                                      pend.append((ci, kc, pT, rpT))
                                      if len(pend) > 2:
                                          ci0, kc0, pT0, rpT0 = pend.pop(0)
                                          mm(po[0:65, :], VG[:, kc0, g, :], pT0[:], ci0 == 0, ci0 == len(kcs) - 1, [rVG, rpT0], [rpo])
                                  while pend:
                                      ci0, kc0, pT0, rpT0 = pend.pop(0)
                                      mm(po[0:65, :], VG[:, kc0, g, :], pT0[:], ci0 == 0, ci0 == len(kcs) - 1, [rVG, rpT0], [rpo])
                                  rd, rrd = rdring.next()
                                  osb, rosb = oring.next()
                                  obf, robf = obring.next()
                                  S.I("dve", "reciprocal", [rpo], [rrd], out=rd[64:65, :], in_=po[64:65, :])
                                  cp("act", osb[:], po[0:64, :], [rpo], [rosb])
                                  pb, rpb = rotB.next()
                                  mm(pb[0:64, :], ones[64:65, 0:64], rd[64:65, :], True, True, [rones, rrd], [rpb])
                                  tt("dve", obf[:], osb[:], pb[0:64, :], ALU.mult, [rosb, rpb], [robf])
                                  for j in range(2):
                                      hd = 2 * g + j
                                      S.D(OT[hd * 64:(hd + 1) * 64, q0:q0 + 256], obf[:, j * 256:(j + 1) * 256], reads=[robf], writes=[Res("ot")], queue="pool")
                          while e0_sched:
                              e0_step()
                          scring = Ring(es, "sc", [128, 640], F32, 2)
                          pfring = Ring(es, "pf", [128, 640], F32, 2)
                          pnring = Ring(es, "pn", [128, 640], BF16, 2)
                          ptring = Ring(es, "ptT", [128, 5, 128], BF16, 2)
                          mring = Ring(es, "mx", [128, 8], F32, 4)
                          owring = Ring(es, "ow", [64, 4, 128], BF16, 2)
                          rotA = Rot([0, 1])
                          rotB2 = Rot([2, 3])
                          rotP = Rot([4, 5])
                          rotW = Rot([6, 7])
                          qtiles = list(range(2, NT)) if last else list(range(NT))
                          pend_w = []
                          for ti in qtiles:
                              tcols = slice(ti * 128, (ti + 1) * 128)
                              n = ti - 2
                              band = [] if ti < 2 else [b for b in (n - 1, n, n + 1) if 0 <= b < 32]
                              bw = 128 * len(band)
                              Wd = bw + 256
                              ktiles = [b + 2 for b in band] + [0, 1]
                              pow_, rpow = rotW.next()
                              ow, row = owring.next()
                              for hd in range(4):
                                  g, j = hd // 2, hd % 2
                                  gs = slice(g * 64, (g + 1) * 64)
                                  sc, rsc = scring.next()
                                  if band:
                                      pA, rpA = rotA.next()
                                      k0 = (band[0] + 2) * 128
                                      mm(pA[:, 0:bw], QWT[gs, j, tcols], KWT[gs, k0:k0 + bw], True, True, [rQWT, rKWT], [rpA])
                                      m0 = 0 if band[0] == n - 1 else 128
                                      tt("dve", sc[:, 0:bw], pA[:, 0:bw], WM[:, m0:m0 + bw], ALU.add, [rpA, rWM], [rsc])
                                  pB, rpB = rotB2.next()
                                  mm(pB[:, 0:256], QWT[gs, j, tcols], KWT[gs, 0:256], True, True, [rQWT, rKWT], [rpB])
                                  cp("act", sc[:, bw:bw + 256], pB[:, 0:256], [rpB], [rsc])
                                  mx, rmx = mring.next()
                                  red(mx[:, 0:1], sc[:, 0:Wd], ALU.max, [rsc], [rmx])
                                  stt(mx[:, 1:2], mx[:, 0:1], 0.125, SK[:, hd:hd + 1], ALU.mult, ALU.max, [rmx, rSK], [rmx])
                                  ts("dve", mx[:, 2:3], mx[:, 1:2], -1.0, None, ALU.mult, None, [rmx], [rmx])
                                  pf, rpf = pfring.next()
                                  act(pf[:, 0:Wd], sc[:, 0:Wd], AF.Exp, [rsc, rmx], [rpf, rmx], scale=0.125, bias=mx[:, 2:3], accum_out=mx[:, 3:4])
                                  act(mx[:, 4:5], SK[:, hd:hd + 1], AF.Exp, [rSK, rmx], [rmx], scale=1.0, bias=mx[:, 2:3])
                                  tt("dve", mx[:, 5:6], mx[:, 3:4], mx[:, 4:5], ALU.add, [rmx], [rmx])
                                  S.I("dve", "reciprocal", [rmx], [rmx], out=mx[:, 6:7], in_=mx[:, 5:6])
                                  pn, rpn = pnring.next()
                                  ts("dve", pn[:, 0:Wd], pf[:, 0:Wd], mx[:, 6:7], None, ALU.mult, None, [rpf, rmx], [rpn])
                                  def stage_b(pn=pn, rpn=rpn, Wd=Wd, ktiles=ktiles, g=g, hd=hd, pow_=pow_, rpow=rpow, ow=ow, row=row, tcols=tcols):
                                      ptp, rptp = rotP.next()
                                      ptv = bfv(ptp)
                                      nch = Wd // 128
                                      for cidx in range(nch):
                                          tp(ptv[:, cidx * 128:(cidx + 1) * 128], pn[:, cidx * 128:(cidx + 1) * 128], identb[:], [rpn, ridentb], [rptp])
                                      ptT, rptT = ptring.next()
                                      cp("act", ptT[:, 0:nch, :], ptv[:, 0:nch * 128].rearrange("p (c t) -> p c t", t=128), [rptp], [rptT])
                                      for cidx in range(nch):
                                          kt = ktiles[cidx]
                                          mm(pow_[0:64, hd * 128:(hd + 1) * 128], VW[:, kt, g, :], ptT[:, cidx, :], cidx == 0, cidx == nch - 1, [rVW, rptT], [rpow])
                                      if hd == 3:
                                          cp("dve", ow[:].rearrange("p h t -> p (h t)"), pow_[0:64, :], [rpow], [row])
                                          S.D(OT[256:512, tcols].rearrange("(h d) t -> d h t", d=64), ow[:], reads=[row], writes=[Res("ot")], queue="pool")
                                  for fn_ in pend_w:
                                      fn_()
                                  pend_w[:] = [stage_b]
                          for fn_ in pend_w:
                              fn_()
                S.barrier()
                if stop_after == "D":
                    break
                with ExitStack() as es:
                    rot = Rot(range(8))
                    rotp = Rot([0, 1])
                    rots_d = [Rot([2, 3, 4]), Rot([5, 6, 7])]
                    PB, rPB = SB(es, "PB", [128, 11, 512])
                    S.D(PB[:, 0:2, :].rearrange("p a b -> p (a b)"), I["rwkv_w0"][l:l + 1].rearrange("o a b -> o (a b)").partition_broadcast(128), writes=[rPB])
                    S.D(PB[:, 2:4, :].rearrange("p a b -> p (a b)"), I["rwkv_a0"][l:l + 1].rearrange("o a b -> o (a b)").partition_broadcast(128), writes=[rPB])
                    S.D(PB[:, 4, :], I["rwkv_k_k"][l:l + 1, :].partition_broadcast(128), writes=[rPB])
                    S.D(PB[:, 5, :], I["rwkv_k_a"][l:l + 1, :].partition_broadcast(128), writes=[rPB])
                    S.D(PB[:, 7:9, :].rearrange("p a b -> p (a b)"), I["rwkv_r_k"][l:l + 1].rearrange("o a b -> o (a b)").partition_broadcast(128), writes=[rPB])
                    S.D(PB[:, 9, :], I["rwkv_ln_w"][l:l + 1, :].partition_broadcast(128), writes=[rPB])
                    S.D(PB[:, 10, :], I["rwkv_ln_b"][l:l + 1, :].partition_broadcast(128), writes=[rPB])
                    ts("pool", PB[:, 6, :], PB[:, 5, :], -1.0, 1.0, ALU.mult, ALU.add, [rPB], [rPB])
                    LW, rLW = SB(es, "LW", [128, 3, 512])
                    S.D(LW[:, 0, :], I["rwkv_w2"][l], writes=[rLW])
                    S.D(LW[:, 1, :], I["rwkv_a2"][l], writes=[rLW])
                    S.D(LW[:, 2, :], I["rwkv_g2"][l], writes=[rLW])
                    TRI, rTRI = SB(es, "TRI", [128, 4, 128])
                    CHI, rCHI = SB(es, "CHI", [128, 2])
                    MK, rMK = SB(es, "MK", [128, 4, 128])
                    S.D(TRI[:], C["tri"], writes=[rTRI])
                    S.D(CHI[:], C["chunkind"], writes=[rCHI])
                    S.D(MK[:], C["mask4"], writes=[rMK])
                    ucbufs = [SB(es, "ucv_", [128, 1920]) for d_ in range(2)]
                    ucmap = {}
                    F5 = {n: SB(es, "r_" + n, [128, 512]) for n in ("sig", "a", "kk", "kdir", "kka", "E1", "E2", "E3", "E4", "tA", "tB", "tC")}
                    B5 = {n: SB(es, "rb_" + n, [128, 512], BF16) for n in ("RTm", "BTm", "KTm")}
                    lin, rlin = SB(es, "lin", [128, 384])
                    twT, rtwT = SB(es, "twT", [128, 3, 128])
                    sm, rsm = SB(es, "smallr", [128, 64])
                    HB = []
                    for hb_i in range(2):
                        hbd = {"XAR": SB(es, "XAR", [64, 8, 2, 128], BF16), "XB": SB(es, "XB", [64, 8, 128], BF16),
                               "XK": SB(es, "XK", [64, 8, 128], BF16), "RP": SB(es, "RP", [64, 8, 2, 128], BF16),
                               "PL": SB(es, "PL", [64, 8, 2]), "AL": SB(es, "hAL", [128, 512], BF16), "VB": SB(es, "hVB", [128, 512], BF16),
                               "BL": SB(es, "hBL", [128, 512], BF16), "KL": SB(es, "hKL", [128, 512], BF16)}
                        S.I("pool", "memset", [], [hbd["RP"][1]], ap=hbd["RP"][0][:], constant=0.0)
                        HB.append(hbd)
                    SI = []
                    for d_ in range(2):
                        SI.append({"AK2": SB(es, "AK2", [128, 8, 2, 128], BF16),
                                   "GT": [SB(es, "GT%d" % i, [128, 8, 3, 128], BF16) for i in range(2)],
                                   "FF": [SB(es, "FF%d" % i, [128, 8, 128], BF16) for i in range(2)],
                                   "WT": SB(es, "WT", [64, 8, 128], BF16), "X1": SB(es, "X1", [128, 8, 64], BF16),
                                   "Ub": SB(es, "Ub", [128, 8, 64], BF16),
                                   "SF": [SB(es, "SF%d" % i, [64, 8, 64]) for i in range(3)],
                                   "SBF": [SB(es, "SBF%d" % i, [64, 8, 64], BF16) for i in range(3)]})
                    stmp_sh = SB(es, "stmp", [64, 8, 64])
                    ysb_sh = SB(es, "ysb", [128, 512])

                    def F(n):
                        return F5[n]

                    def rwkv_prep(dr, ti, hb):
                        XAR, rXAR = hb["XAR"]
                        XB, rXB = hb["XB"]
                        XK, rXK = hb["XK"]
                        RP, rRP = hb["RP"]
                        PL, rPL = hb["PL"]
                        AL, rAL = hb["AL"]
                        VB, rVB = hb["VB"]
                        BL, rBL = hb["BL"]
                        KL, rKL = hb["KL"]
                        rows = slice(ti * 128, (ti + 1) * 128)
                        first_of_seq = ti in (0, 2)
                        last_of_seq = ti in (1, 33)
                        need_y = not (last and ti < 2)
                        uc, ruc = ucmap[(dr, ti)]
                        r_ = uc[:, 0:512]
                        k_ = uc[:, 512:1024]
                        v_ = uc[:, 1024:1536]
                        yield
                        act(lin[:, 0:128], uc[:, 1536:1664], AF.Tanh, [ruc], [rlin])
                        cp("pool", lin[:, 128:256], uc[:, 1664:1792], [ruc], [rlin])
                        nl = 2
                        if dr == 0:
                            act(lin[:, 256:384], uc[:, 1792:1920], AF.Sigmoid, [ruc], [rlin])
                            nl = 3
                        pt, rpt = rotp.next()
                        for i3 in range(nl):
                            tp(pt[:, i3 * 128:(i3 + 1) * 128], lin[:, i3 * 128:(i3 + 1) * 128], ident[:], [rlin, rident], [rpt])
                        cp("act", twT[:, 0:nl, :].rearrange("p a b -> p (a b)"), pt[:, 0:nl * 128], [rpt], [rtwT])
                        ds_ = slice(dr * 64, (dr + 1) * 64)
                        pz, rpz = rotp.next()
                        mm(pz[:], twT[ds_, 0, :], LW[ds_, 0, :], True, True, [rtwT, rLW], [rpz])
                        pa, rpa = rotp.next()
                        mm(pa[:], twT[ds_, 1, :], LW[ds_, 1, :], True, True, [rtwT, rLW], [rpa])
                        sig, rsig = F("sig")
                        a_, ra_ = F("a")
                        tA, rtA = F("tA")
                        tB, rtB = F("tB")
                        tC, rtC = F("tC")
                        tt("dve", tA[:], pz[:], PB[:, dr, :], ALU.add, [rpz, rPB], [rtA])
                        act(sig[:], tA[:], AF.Sigmoid, [rtA], [rsig])
                        tt("dve", tB[:], pa[:], PB[:, 2 + dr, :], ALU.add, [rpa, rPB], [rtB])
                        act(a_[:], tB[:], AF.Sigmoid, [rtB], [ra_])
                        yield
                        t_incl = dr
                        t_before = 2 if dr == 0 else 3
                        t_after = 3 if dr == 0 else 2
                        E1, rE1 = F("E1")
                        E2, rE2 = F("E2")
                        E3, rE3 = F("E3")
                        E4, rE4 = F("E4")
                        pc, rpc = rotp.next()
                        mm(pc[:], TRI[:, t_incl, :], sig[:], True, True, [rTRI, rsig], [rpc])
                        act(E1[:], pc[:], AF.Exp, [rpc], [rE1])
                        act(E3[:], pc[:], AF.Exp, [rpc], [rE3], scale=-1.0)
                        pc, rpc = rotp.next()
                        mm(pc[:], TRI[:, t_before, :], sig[:], True, True, [rTRI, rsig], [rpc])
                        act(E2[:], pc[:], AF.Exp, [rpc], [rE2])
                        pc, rpc = rotp.next()
                        mm(pc[:], TRI[:, t_after, :], sig[:], True, True, [rTRI, rsig], [rpc])
                        act(E4[:], pc[:], AF.Exp, [rpc], [rE4])
                        ppl, rppl = rotp.next()
                        for h in range(8):
                            mm(ppl[0:64, h * 2:(h + 1) * 2], sig[:, h * 64:(h + 1) * 64], CHI[:], True, True, [rsig, rCHI], [rppl])
                        act(PL[:].rearrange("p h c -> p (h c)"), ppl[0:64, 0:16], AF.Exp, [rppl], [rPL])
                        yield
                        kk, rkk = F("kk")
                        kdir, rkdir = F("kdir")
                        kka, rkka = F("kka")
                        tt("pool", tB[:], k_, PB[:, 4, :], ALU.mult, [ruc, rPB, rtB], [rtB])
                        tt("pool", tC[:], tB[:], tB[:], ALU.mult, [rtB, rtC], [rtC])
                        red(sm[:, 0:8], tC[:].rearrange("p (h d) -> p h d", d=64), ALU.add, [rtC], [rsm])
                        act(sm[:, 8:16], sm[:, 0:8], AF.Sqrt, [rsm], [rsm], scale=1.0)
                        ts("dve", sm[:, 8:16], sm[:, 8:16], 1e-12, None, ALU.max, None, [rsm], [rsm])
                        S.I("dve", "reciprocal", [rsm], [rsm], out=sm[:, 8:16], in_=sm[:, 8:16])
                        tt("dve", kk[:].rearrange("p (h d) -> p h d", d=64), tB[:].rearrange("p (h d) -> p h d", d=64),
                           sm[:, 8:16].unsqueeze(2).to_broadcast([128, 8, 64]), ALU.mult, [rtB, rsm], [rkk])
                        tt("pool", tA[:], a_[:], PB[:, 5, :], ALU.mult, [ra_, rPB, rtA], [rtA])
                        tt("pool", tA[:], tA[:], PB[:, 6, :], ALU.add, [rtA, rPB], [rtA])
                        tt("pool", kdir[:], k_, tA[:], ALU.mult, [ruc, rtA], [rkdir])
                        tt("pool", kka[:], kk[:], a_[:], ALU.mult, [rkk, ra_], [rkka])
                        yield
                        RTm, rRTm = B5["RTm"]
                        BTm, rBTm = B5["BTm"]
                        KTm, rKTm = B5["KTm"]
                        tt("dve", RTm[:], r_, E1[:], ALU.mult, [ruc, rE1], [rRTm])
                        stt(AL[:], kk[:], -1.0, E2[:], ALU.mult, ALU.mult, [rkk, rE2], [rAL])
                        tt("pool", BTm[:], kka[:], E3[:], ALU.mult, [rkka, rE3], [rBTm])
                        tt("pool", KTm[:], kdir[:], E3[:], ALU.mult, [rkdir, rE3], [rKTm])
                        tt("dve", BL[:], kka[:], E4[:], ALU.mult, [rkka, rE4], [rBL])
                        tt("pool", KL[:], kdir[:], E4[:], ALU.mult, [rkdir, rE4], [rKL])
                        cp("act", VB[:], v_, [ruc], [rVB])
                        yield
                        for (src, rsrc, kind) in ((AL, rAL, 0), (RTm, rRTm, 1), (BTm, rBTm, 2), (KTm, rKTm, 3)):
                            pt, rpt = rotp.next()
                            pv = bfv(pt)
                            for h in range(8):
                                tp(pv[0:64, h * 128:(h + 1) * 128], src[:, h * 64:(h + 1) * 64], identb[:], [rsrc, ridentb], [rpt])
                            pv3 = pv[0:64, :].rearrange("p (h t) -> p h t", h=8)
                            if kind == 0:
                                cp("act", XAR[:, :, 0, :], pv3, [rpt], [rXAR])
                            elif kind == 1:
                                cp("act", XAR[:, :, 1, :], pv3, [rpt], [rXAR])
                                cp("act", RP[:, :, 0, 0:64], pv3[:, :, 0:64], [rpt], [rRP])
                                cp("act", RP[:, :, 1, 64:128], pv3[:, :, 64:128], [rpt], [rRP])
                            elif kind == 2:
                                cp("act", XB[:], pv3, [rpt], [rXB])
                            else:
                                cp("act", XK[:], pv3, [rpt], [rXK])
                            yield
                        yield
                        if dr == 0:
                            g1, rg1 = F("E1")
                            g2_, rg2_ = F("E2")
                            aO, raO = F("E3")
                            po_, rpo_ = rotp.next()
                            mm(po_[:], twT[64:128, 1, :], LW[64:128, 1, :], True, True, [rtwT, rLW], [rpo_])
                            tt("dve", tC[:], po_[:], PB[:, 3, :], ALU.add, [rpo_, rPB, rtC], [rtC])
                            act(aO[:], tC[:], AF.Sigmoid, [rtC], [raO])
                            pg, rpg = rotp.next()
                            mm(pg[:], twT[:, 2, :], LW[:, 2, :], True, True, [rtwT, rLW], [rpg])
                            tt("dve", tA[:], r_, kdir[:], ALU.mult, [ruc, rkdir, rtA], [rtA])
                            tt("pool", tA[:], tA[:], PB[:, 7, :], ALU.mult, [rtA, rPB], [rtA])
                            red(sm[:, 16:24], tA[:].rearrange("p (h d) -> p h d", d=64), ALU.add, [rtA], [rsm])
                            tt("pool", tC[:], aO[:], PB[:, 5, :], ALU.mult, [raO, rPB, rtC], [rtC])
                            tt("pool", tC[:], tC[:], PB[:, 6, :], ALU.add, [rtC, rPB], [rtC])
                            tt("pool", tC[:], tC[:], k_, ALU.mult, [rtC, ruc], [rtC])
                            tt("dve", tC[:], tC[:], r_, ALU.mult, [rtC, ruc], [rtC])
                            tt("pool", tC[:], tC[:], PB[:, 8, :], ALU.mult, [rtC, rPB], [rtC])
                            red(sm[:, 24:32], tC[:].rearrange("p (h d) -> p h d", d=64), ALU.add, [rtC], [rsm])
                            tt("dve", sm[:, 16:24], sm[:, 16:24], sm[:, 24:32], ALU.add, [rsm], [rsm])
                            tt("dve", tA[:].rearrange("p (h d) -> p h d", d=64), v_.rearrange("p (h d) -> p h d", d=64),
                               sm[:, 16:24].unsqueeze(2).to_broadcast([128, 8, 64]), ALU.mult, [ruc, rsm, rtA], [rtA])
                            tt("pool", tA[:], tA[:], PB[:, 10, :], ALU.add, [rtA, rPB], [rtA])
                            tt("dve", g2_[:], tA[:], pg[:], ALU.mult, [rtA, rpg], [rg2_])
                            tt("dve", g1[:], PB[:, 9, :], pg[:], ALU.mult, [rPB, rpg], [rg1])
                            S.D(G1D[rows, :], g1[:], reads=[rg1], writes=[rG[ti]])
                            S.D(G2D[rows, :], g2_[:], reads=[rg2_], writes=[rG[ti]])
                    def rwkv_solve(dr, ti, sbox, hb):
                        XAR, rXAR = hb["XAR"]
                        XB, rXB = hb["XB"]
                        XK, rXK = hb["XK"]
                        RP, rRP = hb["RP"]
                        PL, rPL = hb["PL"]
                        AL, rAL = hb["AL"]
                        VB, rVB = hb["VB"]
                        BL, rBL = hb["BL"]
                        KL, rKL = hb["KL"]
                        rows = slice(ti * 128, (ti + 1) * 128)
                        need_y = not (last and ti < 2)
                        si = sbox[0]
                        sd_ = SI[dr]
                        rots = rots_d[dr]
                        AK2, rAK2 = sd_["AK2"]
                        GT = sd_["GT"]
                        FF = sd_["FF"]
                        WT, rWT = sd_["WT"]
                        X1, rX1 = sd_["X1"]
                        Ub, rUb = sd_["Ub"]
                        stmp, rstmp = stmp_sh
                        SF = sd_["SF"]
                        SBF = sd_["SBF"]
                        m_s = 0 if dr == 0 else 2
                        m_i = 1 if dr == 0 else 3
                        m_n = 2 if dr == 0 else 0
                        G0, rG0 = GT[0]
                        mk2 = MK[:, 2 * dr:2 * dr + 2, :].unsqueeze(1).to_broadcast([128, 2, 2, 128])
                        for hp in range(4):
                            hs = slice(2 * hp, 2 * hp + 2)
                            for (lhs, rl, dst, rdst) in ((XB, rXB, G0[:, hs, 0:3:2, :], rG0), (XK, rXK, AK2[:, hs, :, :], rAK2)):
                                pq, rpq = rots.next()
                                for hh in range(2):
                                    h = 2 * hp + hh
                                    mm(pq[:, hh * 256:(hh + 1) * 256], lhs[:, h, :], XAR[:, h, :, :].rearrange("p a t -> p (a t)"), True, True, [rl, rXAR], [rpq])
                                pq4 = pq[:].rearrange("p (h a t) -> p h a t", h=2, a=2)
                                tt("dve", dst, pq4, mk2, ALU.mult, [rpq, rMK], [rdst])
                        yield
                        F0, rF0 = FF[0]
                        for hq in range(2):
                            pq, rpq = rots.next()
                            for hh in range(4):
                                h = 4 * hq + hh
                                mm(pq[:, hh * 128:(hh + 1) * 128], XAR[:, h, 0, :], XB[:, h, :], True, True, [rXAR, rXB], [rpq])
                            tt("dve", F0[:, 4 * hq:4 * hq + 4, :], pq[:].rearrange("p (h t) -> p h t", h=4), MK[:, m_n, :].unsqueeze(1).to_broadcast([128, 4, 128]), ALU.mult, [rpq, rMK], [rF0])
                        cp("pool", G0[:, :, 1, :], identb[:].unsqueeze(1).to_broadcast([128, 8, 128]), [ridentb], [rG0])
                        yield
                        for s_ in range(6):
                            cur, nxt = s_ % 2, 1 - s_ % 2
                            Gc, rGc = GT[cur]
                            Gn, rGn = GT[nxt]
                            Fc, rFc = FF[cur]
                            Fn, rFn = FF[nxt]
                            if s_ < 5:
                                for hq in range(2):
                                    pq, rpq = rots.next()
                                    for hh in range(4):
                                        h = 4 * hq + hh
                                        mm(pq[:, hh * 128:(hh + 1) * 128], Gc[:, h, 0, :], Fc[:, h, :], True, True, [rGc, rFc], [rpq])
                                    cp("act", Fn[:, 4 * hq:4 * hq + 4, :], pq[:].rearrange("p (h t) -> p h t", h=4), [rpq], [rFn])
                                for hp in range(4):
                                    hs = slice(2 * hp, 2 * hp + 2)
                                    pq, rpq = rots.next()
                                    for hh in range(2):
                                        h = 2 * hp + hh
                                        mm(pq[:, hh * 256:(hh + 1) * 256], Fc[:, h, :], Gc[:, h, 0:2, :].rearrange("p a t -> p (a t)"), True, True, [rFc, rGc], [rpq])
                                    pq4 = pq[:].rearrange("p (h a t) -> p h a t", h=2, a=2)
                                    cp("act", Gn[:, hs, 0, :], pq4[:, :, 0, :], [rpq], [rGn])
                                    tt("dve", Gn[:, hs, 1, :], pq4[:, :, 1, :], Gc[:, hs, 1, :], ALU.add, [rpq, rGc], [rGn])
                            else:
                                for hq in range(2):
                                    pq, rpq = rots.next()
                                    for hh in range(4):
                                        h = 4 * hq + hh
                                        mm(pq[:, hh * 128:(hh + 1) * 128], Fc[:, h, :], Gc[:, h, 1, :], True, True, [rFc, rGc], [rpq])
                                    tt("dve", Gn[:, 4 * hq:4 * hq + 4, 1, :], pq[:].rearrange("p (h t) -> p h t", h=4), Gc[:, 4 * hq:4 * hq + 4, 1, :], ALU.add, [rpq, rGc], [rGn])
                            yield
                        TTt, rTT = GT[0]
                        for hq in range(2):
                            pq, rpq = rots.next()
                            for hh in range(4):
                                h = 4 * hq + hh
                                mm(pq[0:64, hh * 128:(hh + 1) * 128], AL[:, h * 64:(h + 1) * 64], TTt[:, h, 1, :], True, True, [rAL, rTT], [rpq])
                            cp("act", WT[:, 4 * hq:4 * hq + 4, :], pq[0:64, :].rearrange("p (h t) -> p h t", h=4), [rpq], [rWT])
                        pq, rpq = rots.next()
                        for h in range(8):
                            mm(pq[:, h * 64:(h + 1) * 64], AK2[:, h, 0, :], VB[:, h * 64:(h + 1) * 64], True, True, [rAK2, rVB], [rpq])
                        cp("dve", X1[:].rearrange("p h v -> p (h v)"), pq[:], [rpq], [rX1])
                        yield
                        sidx = {}
                        for c in ((0, 1) if dr == 0 else (1, 0)):
                            crow = slice(c * 64, (c + 1) * 64)
                            M = 64 if c == 0 else 128
                            S0f, rS0f = SF[si]
                            S0b, rS0b = SBF[si]
                            sn = (si + 1) % 3
                            S1f, rS1f = SF[sn]
                            S1b, rS1b = SBF[sn]
                            sidx[c] = si
                            pu, rpu = rots.next()
                            for h in range(8):
                                mm(pu[0:M, h * 64:(h + 1) * 64], WT[:, h, 0:M], S0b[:, h, :], True, False, [rWT, rS0b], [rpu])
                                if c == 0:
                                    mm(pu[0:M, h * 64:(h + 1) * 64], TTt[crow, h, 1, 0:M], X1[crow, h, :], False, True, [rTT, rX1], [rpu])
                                else:
                                    mm(pu[0:M, h * 64:(h + 1) * 64], TTt[:, h, 1, 0:M], X1[:, h, :], False, True, [rTT, rX1], [rpu])
                            cp("act", Ub[crow, :, :].rearrange("p h v -> p (h v)"), pu[crow, :], [rpu], [rUb])
                            psn, rpsn = rots.next()
                            for h in range(8):
                                mm(psn[0:64, h * 64:(h + 1) * 64], BL[crow, h * 64:(h + 1) * 64], Ub[crow, h, :], True, False, [rBL, rUb], [rpsn])
                                mm(psn[0:64, h * 64:(h + 1) * 64], KL[crow, h * 64:(h + 1) * 64], VB[crow, h * 64:(h + 1) * 64], False, True, [rKL, rVB], [rpsn])
                            tt("pool", stmp[:], S0f[:], PL[:, :, c:c + 1].to_broadcast([64, 8, 64]), ALU.mult, [rS0f, rPL], [rstmp])
                            tt("dve", S1f[:].rearrange("p h v -> p (h v)"), stmp[:].rearrange("p h v -> p (h v)"), psn[0:64, :], ALU.add, [rstmp, rpsn], [rS1f])
                            cp("act", S1b[:], S1f[:], [rS1f], [rS1b])
                            si = sn
                            yield
                        if need_y:
                            py, rpy = rots.next()
                            for h in range(8):
                                hc = slice(h * 64, (h + 1) * 64)
                                mm(py[:, hc], RP[:, h, 0, :], SBF[sidx[0]][0][:, h, :], True, False, [rRP, SBF[sidx[0]][1]], [rpy])
                                mm(py[:, hc], RP[:, h, 1, :], SBF[sidx[1]][0][:, h, :], False, False, [rRP, SBF[sidx[1]][1]], [rpy])
                                mm(py[:, hc], GT[0][0][:, h, 2, :], Ub[:, h, :], False, False, [GT[0][1], rUb], [rpy])
                                mm(py[:, hc], AK2[:, h, 1, :], VB[:, hc], False, True, [rAK2, rVB], [rpy])
                            ysb, rysb = ysb_sh
                            cp("act", ysb[:], py[:], [rpy], [rysb])
                            S.D(YD[dr][rows, :], ysb[:], reads=[rysb], writes=[rY[dr][ti]])
                        sbox[0] = si

                    def chain(dr):
                        order = list(range(NT)) if dr == 0 else [1, 0] + list(range(NT - 1, 1, -1))
                        if KT_LIMIT is not None:
                            order = [t_ for t_ in order if t_ < KT_LIMIT - 1]
                        SF0, SBF0 = SI[dr]["SF"][0], SI[dr]["SBF"][0]
                        S.I("pool", "memset", [], [SF0[1]], ap=SF0[0][:], constant=0.0)
                        S.I("pool", "memset", [], [SBF0[1]], ap=SBF0[0][:], constant=0.0)
                        sbox = [0]

                        def uc_load(t_):
                            ub_, rub_ = ucbufs[dr]
                            S.D(ub_[:], UC[t_ * 128:(t_ + 1) * 128, :], reads=[rUC[t_]], writes=[rub_])
                            ucmap[(dr, t_)] = (ub_, rub_)
                        uc_load(order[0])
                        for oi, ti in enumerate(order):
                            while prep_lock[0] is not None and prep_lock[0] != dr:
                                yield
                            prep_lock[0] = dr
                            for _ in rwkv_prep(dr, ti, HB[dr]):
                                yield
                            prep_lock[0] = None
                            if oi + 1 < len(order):
                                uc_load(order[oi + 1])
                            for _ in rwkv_solve(dr, ti, sbox, HB[dr]):
                                yield

                    prep_lock = [None]
                    chains = [chain(0), chain(1)]
                    alive = [True, True]
                    while any(alive):
                        for ci_ in range(2):
                            if alive[ci_]:
                                try:
                                    next(chains[ci_])
                                except StopIteration:
                                    alive[ci_] = False
                S.barrier()
                if stop_after == "E":
                    break
                ftiles = list(range(2, NT)) if last else list(range(NT))
                if KT_LIMIT is not None:
                    ftiles = [t_ for t_ in ftiles if t_ < KT_LIMIT - 1]
                with ExitStack() as es:
                    rot = Rot(range(8))
                    Wout, rWout = SB(es, "Wout", [128, 8, 1024], BF16)
                    S.D(Wout[:], I["w_out"][l].rearrange("(k p) n -> p k n", p=128), writes=[rWout], queue="pool")
                    oaring = Ring(es, "oTa", [128, 4, 128], BF16, 3)
                    orring = Ring(es, "oTr", [128, 4, 128], BF16, 2)
                    yring = Ring(es, "yio", [128, 4, 512], F32, 3)
                    h1ring = Ring(es, "h1t", [128, 1024], F32, 2)
                    hring = Ring(es, "htf", [128, 1024], F32, 3)
                    obring = Ring(es, "obf2", [128, 512], BF16, 2)
                    o_, ro_ = SB(es, "o_sum", [128, 512])
                    oc_, roc_ = SB(es, "o_cen", [128, 512])
                    sq_, rsq_ = SB(es, "o_sq", [128, 512])
                    gs, rgs = SB(es, "gnst", [128, 32])
                    pend_f = []
                    for ti in ftiles:
                        rows = slice(ti * 128, (ti + 1) * 128)
                        tcols = slice(ti * 128, (ti + 1) * 128)
                        which = 1 if ti < 2 else 0
                        oTa, roTa = oaring.next()
                        S.D(oTa[:], OT[0:512, tcols].rearrange("(c p) t -> p c t", p=128), reads=[], writes=[roTa])
                        yio, ryio = yring.next()
                        S.D(yio[:, 0, :], YD[0][rows, :], reads=[rY[0][ti]], writes=[ryio])
                        S.D(yio[:, 1, :], YD[1][rows, :], reads=[rY[1][ti]], writes=[ryio])
                        S.D(yio[:, 2, :], G1D[rows, :], reads=[rG[ti]], writes=[ryio])
                        S.D(yio[:, 3, :], G2D[rows, :], reads=[rG[ti]], writes=[ryio])
                        ht, rht = hring.next()
                        S.D(ht[:], hsrc(l, ti), reads=[rH[ti]], writes=[rht])
                        v3 = lambda t_: t_[:].rearrange("p (h d) -> p h d", d=64)
                        tt("pool", o_[:], yio[:, 0, :], yio[:, 1, :], ALU.add, [ryio], [ro_])
                        red(gs[:, 0:8], v3(o_), ALU.add, [ro_], [rgs])
                        ts("dve", gs[:, 8:16], gs[:, 0:8], 1.0 / 64.0, None, ALU.mult, None, [rgs], [rgs])
                        tt("dve", v3(oc_), v3(o_), gs[:, 8:16].unsqueeze(2).to_broadcast([128, 8, 64]), ALU.subtract, [ro_, rgs], [roc_])
                        tt("pool", sq_[:], oc_[:], oc_[:], ALU.mult, [roc_], [rsq_])
                        red(gs[:, 16:24], v3(sq_), ALU.add, [rsq_], [rgs])
                        act(gs[:, 24:32], gs[:, 16:24], AF.Sqrt, [rgs], [rgs], bias=64e-5, scale=1.0 / 64.0)
                        S.I("dve", "reciprocal", [rgs], [rgs], out=gs[:, 24:32], in_=gs[:, 24:32])
                        tt("dve", v3(sq_), v3(oc_), gs[:, 24:32].unsqueeze(2).to_broadcast([128, 8, 64]), ALU.mult, [roc_, rgs, rsq_], [rsq_])
                        tt("pool", sq_[:], sq_[:], yio[:, 2, :], ALU.mult, [rsq_, ryio], [rsq_])
                        ob, rob = obring.next()
                        tt("pool", ob[:], sq_[:], yio[:, 3, :], ALU.add, [rsq_, ryio], [rob])
                        pt, rpt = rot.next()
                        pv = bfv(pt)
                        for k in range(4):
                            tp(pv[:, k * 128:(k + 1) * 128], ob[:, k * 128:(k + 1) * 128], identb[:], [rob, ridentb], [rpt])
                        oTr, roTr = orring.next()
                        cp("act", oTr[:].rearrange("p c t -> p (c t)"), pv[:, 0:512], [rpt], [roTr])
                        def stage2(oTa=oTa, roTa=roTa, oTr=oTr, roTr=roTr, ht=ht, rht=rht, rows=rows, which=which, ti=ti):
                            h1, rh1 = h1ring.next()
                            for half in range(2):
                                hc = slice(half * 512, (half + 1) * 512)
                                pq, rpq = rot.next()
                                for k in range(8):
                                    lhs = oTa[:, k, :] if k < 4 else oTr[:, k - 4, :]
                                    mm(pq[:], lhs, Wout[:, k, hc], k == 0, k == 7, [roTa, roTr, rWout], [rpq])
                                tt("dve", h1[:, hc], pq[:], GM[:, 0, which, hc], ALU.mult, [rpq, rGM], [rh1])
                            tt("pool", h1[:], h1[:], ht[:], ALU.add, [rh1, rht], [rh1])
                            S.D(H1[rows, :], h1[:], reads=[rh1], writes=[rH1[ti]], queue="pool")
                        for fn_ in pend_f:
                            fn_()
                        pend_f[:] = [stage2]
                    for fn_ in pend_f:
                        fn_()
                S.barrier()
                if stop_after == "F1":
                    break
                with ExitStack() as es:
                    rot = Rot(range(8))
                    GTL = 8 if last else 9
                    WR, rWR = SB(es, "WR", [128, 8, 16])
                    S.D(WR[:], I["w_router"].rearrange("(k p) e -> p k e", p=128), writes=[rWR])
                    RB, rRB = SB(es, "RB", [128, 16])
                    S.D(RB[:], I["router_bias"].partition_broadcast(128), writes=[rRB])
                    if last:
                        FNG, rFNG = SB(es, "FNG", [128, 1024])
                        S.D(FNG[:], I["final_norm_g"].partition_broadcast(128), writes=[rFNG])
                    wring = Ring(es, "wexp", [128, 8, 1024], BF16, 5)
                    FT, rFT = SB(es, "FT", [128, 8, GTL * 128], BF16)
                    YA, rYA = SB(es, "YA", [128, GTL, 1024])
                    GATE, rGATE = SB(es, "GATE", [128, GTL, 16])
                    ACTT, rACTT = SB(es, "ACTT", [128, 8, 512], BF16)
                    rings = {"junk": Ring(es, "junk2", [128, 1024], F32, 2), "st": Ring(es, "st2", [128, 4], F32, 2),
                             "xnf": Ring(es, "xnf", [128, 1024], F32, 2)}
                    xfring = Ring(es, "xf", [128, 8, 128], F32, 2)
                    hring = Ring(es, "ht2", [128, 1024], F32, 2)
                    sgring = Ring(es, "sg", [128, 512], F32, 2)
                    rtring = Ring(es, "rt", [128, 160], F32, 2)
                    groups = [ftiles[i:i + GTL] for i in range(0, len(ftiles), GTL)]
                    for grp in groups:
                        for gi, ti in enumerate(grp):
                            rows = slice(ti * 128, (ti + 1) * 128)
                            which = 1 if ti < 2 else 0
                            ht, rht = hring.next()
                            xf, rxf = xfring.next()
                            rt, rrt = rtring.next()
                            S.D(ht[:], H1[rows, :], reads=[rH1[ti]], writes=[rht])
                            norm_mod_T(rings, ht, rht, which, 2, FT[:, :, gi * 128:(gi + 1) * 128], rFT, rot, fp32=True, xf=xf, rxf=rxf)
                            pl_, rpl_ = rot.next()
                            for k in range(8):
                                mm(pl_[:, 0:16], xf[:, k, :], WR[:, k, :], k == 0, k == 7, [rxf, rWR], [rpl_])
                            sc_ = rt[:, 0:16]
                            bi = rt[:, 16:32]
                            bi3 = bi.rearrange("p (g e) -> p g e", g=4)
                            act(sc_, pl_[:, 0:16], AF.Sigmoid, [rpl_], [rrt])
                            tt("dve", bi, sc_, RB[:], ALU.add, [rrt, rRB], [rrt])
                            red(rt[:, 32:36], bi3, ALU.max, [rrt], [rrt])
                            eq3 = rt[:, 48:64].rearrange("p (g e) -> p g e", g=4)
                            tt("dve", eq3, bi3, rt[:, 32:36].unsqueeze(2).to_broadcast([128, 4, 4]), ALU.is_equal, [rrt], [rrt])
                            b23 = rt[:, 64:80].rearrange("p (g e) -> p g e", g=4)
                            stt(b23, eq3, -10.0, bi3, ALU.mult, ALU.add, [rrt], [rrt])
                            red(rt[:, 36:40], b23, ALU.max, [rrt], [rrt])
                            tt("dve", rt[:, 40:44], rt[:, 32:36], rt[:, 36:40], ALU.add, [rrt], [rrt])
                            red(rt[:, 44:45], rt[:, 40:44], ALU.max, [rrt], [rrt])
                            ts("dve", rt[:, 80:84], rt[:, 40:44], rt[:, 44:45], None, ALU.is_equal, None, [rrt], [rrt])
                            ts("dve", rt[:, 80:84], rt[:, 80:84], -1.0, 10.0, ALU.add, ALU.mult, [rrt], [rrt])
                            mk3 = rt[:, 96:112].rearrange("p (g e) -> p g e", g=4)
                            tt("dve", mk3, bi3, rt[:, 80:84].unsqueeze(2).to_broadcast([128, 4, 4]), ALU.add, [rrt], [rrt])
                            red(rt[:, 45:46], rt[:, 96:112], ALU.max, [rrt], [rrt])
                            ts("dve", rt[:, 112:128], rt[:, 96:112], rt[:, 45:46], None, ALU.is_equal, None, [rrt], [rrt])
                            stt(rt[:, 128:144], rt[:, 112:128], -10.0, rt[:, 96:112], ALU.mult, ALU.add, [rrt], [rrt])
                            red(rt[:, 46:47], rt[:, 128:144], ALU.max, [rrt], [rrt])
                            ts("dve", rt[:, 144:160], rt[:, 128:144], rt[:, 46:47], None, ALU.is_equal, None, [rrt], [rrt])
                            tt("dve", rt[:, 112:128], rt[:, 112:128], rt[:, 144:160], ALU.add, [rrt], [rrt])
                            tt("dve", rt[:, 112:128], rt[:, 112:128], sc_, ALU.mult, [rrt], [rrt])
                            red(rt[:, 47:48], rt[:, 112:128], ALU.add, [rrt], [rrt])
                            S.I("dve", "reciprocal", [rrt], [rrt], out=rt[:, 47:48], in_=rt[:, 47:48])
                            ts("dve", GATE[:, gi, :], rt[:, 112:128], rt[:, 47:48], None, ALU.mult, None, [rrt], [rGATE])
                        ntg = len(grp)
                        chunks = [(c0, min(4, ntg - c0)) for c0 in range(0, ntg, 4)]
                        for e_ in range(16):
                            Wg, rWg = wring.next()
                            S.D(Wg[:], I["e_gate"][l, e_].rearrange("(k p) n -> p k n", p=128), writes=[rWg], queue="pool")
                            Wu, rWu = wring.next()
                            S.D(Wu[:], I["e_up"][l, e_].rearrange("(k p) n -> p k n", p=128), writes=[rWu], queue="pool")
                            Wd, rWd = wring.next()
                            S.D(Wd[:], I["e_down"][l, e_].rearrange("(k p) n -> p k n", p=128), writes=[rWd], queue="pool")
                            for (c0, nct) in chunks:
                                ncol = nct * 128
                                tk = slice(c0 * 128, c0 * 128 + ncol)
                                for fc in range(8):
                                    fs = slice(fc * 128, (fc + 1) * 128)
                                    pg, rpg = rot.next()
                                    for k in range(8):
                                        mm(pg[:, 0:ncol], Wg[:, k, fs], FT[:, k, tk], k == 0, k == 7, [rWg, rFT], [rpg])
                                    pu_, rpu_ = rot.next()
                                    for k in range(8):
                                        mm(pu_[:, 0:ncol], Wu[:, k, fs], FT[:, k, tk], k == 0, k == 7, [rWu, rFT], [rpu_])
                                    sg, rsg = sgring.next()
                                    act(sg[:, 0:ncol], pg[:, 0:ncol], AF.Silu, [rpg], [rsg])
                                    tt("dve", ACTT[:, fc, 0:ncol], sg[:, 0:ncol], pu_[:, 0:ncol], ALU.mult, [rsg, rpu_], [rACTT])
                                for t4 in range(nct):
                                    gi = c0 + t4
                                    for half in range(2):
                                        hc = slice(half * 512, (half + 1) * 512)
                                        py, rpy = rot.next()
                                        for fc in range(8):
                                            mm(py[:], ACTT[:, fc, t4 * 128:(t4 + 1) * 128], Wd[:, fc, hc], fc == 0, fc == 7, [rACTT, rWd], [rpy])
                                        if e_ == 0:
                                            ts("dve", YA[:, gi, hc], py[:], GATE[:, gi, e_:e_ + 1], None, ALU.mult, None, [rpy, rGATE], [rYA])
                                        else:
                                            stt(YA[:, gi, hc], py[:], GATE[:, gi, e_:e_ + 1], YA[:, gi, hc], ALU.mult, ALU.add, [rpy, rGATE, rYA], [rYA])
                        for gi, ti in enumerate(grp):
                            rows = slice(ti * 128, (ti + 1) * 128)
                            which = 1 if ti < 2 else 0
                            ht, rht = hring.next()
                            S.D(ht[:], H1[rows, :], reads=[rH1[ti]], writes=[rht])
                            tt("dve", YA[:, gi, :], YA[:, gi, :], GM[:, 1, which, :], ALU.mult, [rYA, rGM], [rYA])
                            tt("dve", ht[:], ht[:], YA[:, gi, :], ALU.add, [rht, rYA], [rht])
                            if not last:
                                S.D(H[rows, :], ht[:], reads=[rht], writes=[rH[ti]], queue="pool")
                            else:
                                junk, rjunk = rings["junk"].next()
                                st, rst = rings["st"].next()
                                act(junk[:], ht[:], AF.Square, [rht], [rjunk, rst], scale=1.0 / 32.0, accum_out=st[:, 0:1])
                                act(st[:, 1:2], st[:, 0:1], AF.Sqrt, [rst], [rst], bias=1e-6, scale=1.0)
                                S.I("dve", "reciprocal", [rst], [rst], out=st[:, 2:3], in_=st[:, 1:2])
                                ts("dve", junk[:], ht[:], st[:, 2:3], None, ALU.mult, None, [rht, rst, rjunk], [rjunk])
                                tt("pool", junk[:], junk[:], FNG[:], ALU.mult, [rjunk, rFNG], [rjunk])
                                S.D(OUT[(ti - 2) * 128:(ti - 1) * 128, :], junk[:], reads=[rjunk], writes=[rOUT], queue="pool")
                S.barrier()
                if stop_after == "F2":
                    break
        S.finish([rOUT, rOT, rU] + rH + rH1 + rUC + rY[0] + rY[1] + rG + ([rdd] if dbg else []))
        S.replay()
    return nc


_NC_CACHE = {}


def make_in_maps(inputs, batches):
    consts = host_consts()
    in_maps = []
    for b in batches:
        m = {}
        for k, shp in IN_SHAPES.items():
            a = np.asarray(inputs[k], dtype=np.float32)
            if k in ("x", "ctx"):
                a = a[b]
            elif k == "c":
                a = a[b:b + 1]
            m[k] = np.ascontiguousarray(a.reshape(shp))
        for k, v in consts.items():
            m["k_" + k] = v
        in_maps.append(m)
    return in_maps


def kernel(**inputs):
    if "nc" not in _NC_CACHE:
        _NC_CACHE["nc"] = build()
    nc = _NC_CACHE["nc"]
    in_maps = make_in_maps(inputs, range(8))
    res = run_bass_kernel_spmd(nc, in_maps, core_ids=list(range(8)))
    return np.stack([r["out"] for r in res.results], axis=0).astype(np.float32)
```
